# Optimizing a Trainium2 kernel written in Bass

```python
import math
import jax, jax.numpy as jnp
from jax import lax
import numpy as np

D_MODEL = 1024
BATCH = 1
SEQ = 16384
DEPTH = 2

CHUNK = 64
Q_BLOCK = 128
A_HEADS = 8
A_HEAD_DIM = 64
A_WIDTH = A_HEADS * A_HEAD_DIM
IDX_HEADS = 8
IDX_DIM = 64
TOPK_MAX = 256
B_WIDTH = 512
B_GROUPS = 8
SHORT_CONV = 3
C_WIDTH = 512
POOL_WINDOWS = (2, 4, 8, 16)
C_GROUPS = len(POOL_WINDOWS)
C_GROUP_DIM = C_WIDTH // C_GROUPS
D_HEADS = 8
D_NOPE = 64
D_ROPE = 32
D_V = 64
Q_LORA = 384
KV_LORA = 256
ROPE_BASE = 10000.0
D_FF = 2816
FFN_CONV = 3
LN_EPS = 1e-5
RMS_EPS = 1e-6
DN_ALPHA = (2 * DEPTH) ** 0.25
DN_BETA = (8 * DEPTH) ** -0.25
NEG = -1e30

EVEN_SIZES = (A_WIDTH, A_WIDTH, A_WIDTH, IDX_HEADS * IDX_DIM, IDX_DIM, IDX_HEADS,
              B_WIDTH, B_WIDTH, B_WIDTH)
EVEN_IN = sum(EVEN_SIZES)
EVEN_OUT = A_WIDTH + B_WIDTH
ODD_SIZES = (C_WIDTH, Q_LORA, KV_LORA, D_ROPE)
ODD_IN = sum(ODD_SIZES)
ODD_OUT = C_WIDTH + D_HEADS * D_V
N_EVEN = (DEPTH + 1) // 2
N_ODD = DEPTH // 2

kernel_name = "hybrid_streaming_dsa_shortconv_pool_mla"


def _split(z, sizes):
    offs = []
    acc = 0
    for s in sizes[:-1]:
        acc += s
        offs.append(acc)
    return jnp.split(z, offs, axis=-1)


def layer_norm(x, g, b):
    x32 = x.astype(jnp.float32)
    mu = jnp.mean(x32, axis=-1, keepdims=True)
    var = jnp.mean(jnp.square(x32 - mu), axis=-1, keepdims=True)
    y = (x32 - mu) * lax.rsqrt(var + LN_EPS)
    return (y * g.astype(jnp.float32) + b.astype(jnp.float32)).astype(x.dtype)


def rms_norm(x, g):
    x32 = x.astype(jnp.float32)
    y = x32 * lax.rsqrt(jnp.mean(jnp.square(x32), axis=-1, keepdims=True) + RMS_EPS)
    return (y * g.astype(jnp.float32)).astype(x.dtype)


def causal_dwconv(x, w):
    k_w = w.shape[0]
    seq = x.shape[1]
    xp = jnp.pad(x, ((0, 0), (k_w - 1, 0), (0, 0)))
    y = xp[:, 0:seq] * w[0]
    for k in range(1, k_w):
        y = y + xp[:, k:k + seq] * w[k]
    return y


def chunk_limit(pos):
    return (pos // CHUNK + 1) * CHUNK


def alibi_slopes(n):
    return jnp.asarray([2.0 ** (-8.0 * (i + 1) / n) for i in range(n)], dtype=jnp.float32)


def rope_tables(seq, dim):
    inv = ROPE_BASE ** (-jnp.arange(0, dim, 2, dtype=jnp.float32) / dim)
    ang = jnp.arange(seq, dtype=jnp.float32)[:, None] * inv[None, :]
    return jnp.cos(ang), jnp.sin(ang)


def apply_rope(x, cos, sin):
    half = x.shape[-1] // 2
    x1, x2 = x[..., :half], x[..., half:]
    c = cos[None, :, None, :].astype(x.dtype)
    s = sin[None, :, None, :].astype(x.dtype)
    return jnp.concatenate([x1 * c - x2 * s, x2 * c + x1 * s], axis=-1)


def dsa_attention(q, k, v, q_idx, k_idx, w_idx):
    bsz, seq = q.shape[0], q.shape[1]
    topk = min(TOPK_MAX, seq // 4)
    nblk = seq // Q_BLOCK
    key_pos = jnp.arange(seq)
    slopes = alibi_slopes(A_HEADS)
    w_scaled = w_idx * (IDX_HEADS ** -0.5 * IDX_DIM ** -0.5)
    scale = A_HEAD_DIM ** -0.5

    def block(i):
        start = i * Q_BLOCK
        qb = lax.dynamic_slice_in_dim(q, start, Q_BLOCK, axis=1)
        qib = lax.dynamic_slice_in_dim(q_idx, start, Q_BLOCK, axis=1)
        wb = lax.dynamic_slice_in_dim(w_scaled, start, Q_BLOCK, axis=1)
        qpos = start + jnp.arange(Q_BLOCK)
        limit = chunk_limit(qpos)
        rel = jax.nn.relu(jnp.einsum('bthd,bsd->bths', qib, k_idx))
        score = jnp.einsum('bths,bth->bts', rel, wb)
        admissible = key_pos[None, :] < limit[:, None]
        score = jnp.where(admissible[None], score, NEG)
        _, idx = lax.top_k(score, topk)
        valid = idx < limit[None, :, None]
        k_sel = jax.vmap(lambda kk, ii: kk[ii])(k, idx)
        v_sel = jax.vmap(lambda vv, ii: vv[ii])(v, idx)
        logits = jnp.einsum('bthd,btjhd->bthj', qb, k_sel).astype(jnp.float32) * scale
        dist = jnp.abs(qpos[None, :, None] - idx).astype(jnp.float32)
        logits = logits - slopes[None, None, :, None] * dist[:, :, None, :]
        logits = jnp.where(valid[:, :, None, :], logits, NEG)
        p = jax.nn.softmax(logits, axis=-1).astype(v.dtype)
        return jnp.einsum('bthj,btjhd->bthd', p, v_sel)

    out = lax.map(block, jnp.arange(nblk))
    return jnp.moveaxis(out, 0, 1).reshape(bsz, seq, A_WIDTH)


def multiscale_pool(u, pool_w, pool_scale):
    bsz, seq = u.shape[0], u.shape[1]
    ug = u.reshape(bsz, seq, C_GROUPS, C_GROUP_DIM)
    cs = jnp.cumsum(ug.astype(jnp.float32), axis=1)
    cs = jnp.pad(cs, ((0, 0), (1, 0), (0, 0), (0, 0)))
    pos = jnp.arange(seq)
    means = []
    for g, win in enumerate(POOL_WINDOWS):
        hi = cs[:, 1:, g]
        lo = cs[:, jnp.maximum(pos + 1 - win, 0), g]
        cnt = jnp.minimum(pos + 1, win).astype(jnp.float32)
        means.append((hi - lo) / cnt[None, :, None])
    pooled = jnp.stack(means, axis=2).astype(u.dtype) - ug
    mixed = jnp.einsum('bsgc,gcd->bsgd', pooled, pool_w)
    return mixed.reshape(bsz, seq, C_WIDTH) * pool_scale


def mla_attention(q_lat, kv_lat, k_rope_in, q_norm_g, w_uq, kv_norm_g, w_ukv):
    bsz, seq = q_lat.shape[0], q_lat.shape[1]
    q = (rms_norm(q_lat, q_norm_g) @ w_uq).reshape(bsz, seq, D_HEADS, D_NOPE + D_ROPE)
    q_nope, q_rope = q[..., :D_NOPE], q[..., D_NOPE:]
    kv = (rms_norm(kv_lat, kv_norm_g) @ w_ukv).reshape(bsz, seq, D_HEADS, D_NOPE + D_V)
    k_nope, v = kv[..., :D_NOPE], kv[..., D_NOPE:]
    cos, sin = rope_tables(seq, D_ROPE)
    q_rope = apply_rope(q_rope, cos, sin)
    k_rope = apply_rope(k_rope_in[:, :, None, :], cos, sin)
    qh = jnp.concatenate([q_nope, q_rope], axis=-1)
    kh = jnp.concatenate([k_nope, jnp.broadcast_to(k_rope, (bsz, seq, D_HEADS, D_ROPE))], axis=-1)
    scale = (D_NOPE + D_ROPE) ** -0.5
    key_pos = jnp.arange(seq)
    nblk = seq // Q_BLOCK

    def block(i):
        start = i * Q_BLOCK
        qb = lax.dynamic_slice_in_dim(qh, start, Q_BLOCK, axis=1)
        limit = chunk_limit(start + jnp.arange(Q_BLOCK))
        logits = jnp.einsum('bthd,bshd->bhts', qb, kh).astype(jnp.float32) * scale
        mask = key_pos[None, :] < limit[:, None]
        logits = jnp.where(mask[None, None], logits, NEG)
        p = jax.nn.softmax(logits, axis=-1).astype(v.dtype)
        return jnp.einsum('bhts,bshd->bthd', p, v)

    out = lax.map(block, jnp.arange(nblk))
    return jnp.moveaxis(out, 0, 1).reshape(bsz, seq, D_HEADS * D_V)


def even_mixer(h, w_in, conv_w, w_out):
    bsz, seq = h.shape[0], h.shape[1]
    z = h @ w_in
    q, k, v, qi, ki, wi, bg, cg, xb = _split(z, EVEN_SIZES)
    ya = dsa_attention(q.reshape(bsz, seq, A_HEADS, A_HEAD_DIM),
                       k.reshape(bsz, seq, A_HEADS, A_HEAD_DIM),
                       v.reshape(bsz, seq, A_HEADS, A_HEAD_DIM),
                       qi.reshape(bsz, seq, IDX_HEADS, IDX_DIM), ki, wi)
    yb = bg * causal_dwconv(cg * xb, conv_w)
    return jnp.concatenate([ya, yb], axis=-1) @ w_out


def odd_mixer(h, w_in, pool_w, pool_scale, q_norm_g, w_uq, kv_norm_g, w_ukv, w_out):
    z = h @ w_in
    u, q_lat, kv_lat, k_rope = _split(z, ODD_SIZES)
    yc = multiscale_pool(u, pool_w, pool_scale)
    yd = mla_attention(q_lat, kv_lat, k_rope, q_norm_g, w_uq, kv_norm_g, w_ukv)
    return jnp.concatenate([yc, yd], axis=-1) @ w_out


def conv_ffn(h, w_up, conv_w, w_down):
    u = causal_dwconv(h @ w_up, conv_w)
    val, gate = u[..., :D_FF], u[..., D_FF:]
    return (jax.nn.silu(gate) * val) @ w_down


def setup_inputs(seed: int = 0) -> dict:
    key = jax.random.key(seed)
    ks = iter(jax.random.split(key, 40))
    f32 = jnp.float32

    def nrm(shape, scale):
        return jax.random.normal(next(ks), shape, f32) * scale

    d = D_MODEL
    x = nrm((BATCH, SEQ, d), 1.0)
    c = nrm((BATCH, d), 1.0)
    ada_w = nrm((DEPTH, d, 6 * d), 0.1 * d ** -0.5)
    ada_b = nrm((DEPTH, 6 * d), 0.01)
    ln_mix_g = 1.0 + nrm((DEPTH, d), 0.01)
    ln_mix_b = nrm((DEPTH, d), 0.01)
    ln_ffn_g = 1.0 + nrm((DEPTH, d), 0.01)
    ln_ffn_b = nrm((DEPTH, d), 0.01)

    even_col_scale = jnp.concatenate([
        jnp.ones((2 * A_WIDTH,), f32), jnp.full((A_WIDTH,), DN_BETA, f32),
        jnp.ones((EVEN_IN - 3 * A_WIDTH,), f32)])
    ev_w_in = nrm((N_EVEN, d, EVEN_IN), d ** -0.5) * even_col_scale
    ev_conv_w = nrm((N_EVEN, SHORT_CONV, B_WIDTH), SHORT_CONV ** -0.5)
    ev_w_out = nrm((N_EVEN, EVEN_OUT, d), DN_BETA * EVEN_OUT ** -0.5)

    od_w_in = nrm((N_ODD, d, ODD_IN), d ** -0.5)
    pool_w = nrm((N_ODD, C_GROUPS, C_GROUP_DIM, C_GROUP_DIM), C_GROUP_DIM ** -0.5)
    pool_scale = 1.0 + nrm((N_ODD, C_WIDTH), 0.1)
    q_norm_g = 1.0 + nrm((N_ODD, Q_LORA), 0.01)
    w_uq = nrm((N_ODD, Q_LORA, D_HEADS * (D_NOPE + D_ROPE)), Q_LORA ** -0.5)
    kv_norm_g = 1.0 + nrm((N_ODD, KV_LORA), 0.01)
    ukv_col_scale = jnp.tile(jnp.concatenate([jnp.ones((D_NOPE,), f32),
                                              jnp.full((D_V,), DN_BETA, f32)]), D_HEADS)
    w_ukv = nrm((N_ODD, KV_LORA, D_HEADS * (D_NOPE + D_V)), KV_LORA ** -0.5) * ukv_col_scale
    od_w_out = nrm((N_ODD, ODD_OUT, d), DN_BETA * ODD_OUT ** -0.5)

    ffn_w_up = nrm((DEPTH, d, 2 * D_FF), d ** -0.5)
    ffn_conv_w = nrm((DEPTH, FFN_CONV, 2 * D_FF), FFN_CONV ** -0.5)
    ffn_w_down = nrm((DEPTH, D_FF, d), DN_BETA * D_FF ** -0.5)

    return {"x": x, "c": c, "ada_w": ada_w, "ada_b": ada_b,
            "ln_mix_g": ln_mix_g, "ln_mix_b": ln_mix_b, "ln_ffn_g": ln_ffn_g, "ln_ffn_b": ln_ffn_b,
            "ev_w_in": ev_w_in, "ev_conv_w": ev_conv_w, "ev_w_out": ev_w_out,
            "od_w_in": od_w_in, "pool_w": pool_w, "pool_scale": pool_scale,
            "q_norm_g": q_norm_g, "w_uq": w_uq, "kv_norm_g": kv_norm_g, "w_ukv": w_ukv,
            "od_w_out": od_w_out,
            "ffn_w_up": ffn_w_up, "ffn_conv_w": ffn_conv_w, "ffn_w_down": ffn_w_down}


def reference(x, c, ada_w, ada_b, ln_mix_g, ln_mix_b, ln_ffn_g, ln_ffn_b,
              ev_w_in, ev_conv_w, ev_w_out,
              od_w_in, pool_w, pool_scale, q_norm_g, w_uq, kv_norm_g, w_ukv, od_w_out,
              ffn_w_up, ffn_conv_w, ffn_w_down):
    cond = jax.nn.silu(c)
    for l in range(DEPTH):
        mod = (cond @ ada_w[l] + ada_b[l])[:, None, :]
        sh_m, sc_m, g_m, sh_f, sc_f, g_f = jnp.split(mod, 6, axis=-1)
        h = x * (1.0 + sc_m) + sh_m
        if l % 2 == 0:
            e = l // 2
            y = even_mixer(h, ev_w_in[e], ev_conv_w[e], ev_w_out[e])
        else:
            o = l // 2
            y = odd_mixer(h, od_w_in[o], pool_w[o], pool_scale[o], q_norm_g[o], w_uq[o],
                          kv_norm_g[o], w_ukv[o], od_w_out[o])
        x = layer_norm(DN_ALPHA * x + (1.0 + g_m) * y, ln_mix_g[l], ln_mix_b[l])
        h = x * (1.0 + sc_f) + sh_f
        y = conv_ffn(h, ffn_w_up[l], ffn_conv_w[l], ffn_w_down[l])
        x = layer_norm(DN_ALPHA * x + (1.0 + g_f) * y, ln_ffn_g[l], ln_ffn_b[l])
    return x
```

```python
import os
import numpy as np
import ml_dtypes
import concourse.bass as bass
import concourse.mybir as mybir
from concourse.bass_utils import run_bass_kernel_spmd
from contextlib import ExitStack

F32 = mybir.dt.float32
BF16 = mybir.dt.bfloat16
AF = mybir.ActivationFunctionType
ALU = mybir.AluOpType
NPBF = ml_dtypes.bfloat16

NCORES = 8
D = 1024
SEQ = 16384
OWN = SEQ // NCORES
HALO = 128
LT = OWN + HALO
NQT = LT // 128
TT = [(0, 128)] + [(128 + 512 * i, 512) for i in range(4)]
DFF = 2816
NPAIR = DFF // 128
DN_ALPHA = 4.0 ** 0.25
LN_EPS = 1e-5
RMS_EPS = 1e-6
BIGI = 2048.0
BIGS = 30000.0
NBIS = 20
QTILES = [int(v) for v in os.environ["QTILES"].split(",")] if "QTILES" in os.environ else None
MGROUPS = [int(v) for v in os.environ["MGROUPS"].split(",")] if "MGROUPS" in os.environ else None
A_BLKS = [int(v) for v in os.environ.get("A_BLKS", "0,1,2,3,4,5,6,7").split(",")]

ENGINES = ["pe", "act", "dve", "pool", "sp"]
SAME_ENGINE_SYNC = True
SEM_ROLL = 30000


class Op:
    __slots__ = ("eng", "fn", "deps", "signal", "is_dma", "key", "sig", "inc")

    def __init__(self, eng, fn, is_dma=False, key=None):
        self.eng = eng
        self.fn = fn
        self.deps = []
        self.signal = False
        self.is_dma = is_dma
        self.key = key
        self.sig = None
        self.inc = 16


class Sched:
    def __init__(self):
        self.ops = {e: [] for e in ENGINES}
        self.last_writer = {}
        self.readers = {}
        self.last_dma_on_key = {}
        self.key_cnt = {}
        self.nops = 0
        self.bar = []
        self.bar_applied = set(ENGINES)
        self.key_phys = {}

    def barrier(self):
        b = [self.ops[e][-1] for e in ENGINES if self.ops[e]]
        b += list(self.last_dma_on_key.values())
        self.bar = b
        self.bar_applied = set()
        self.key_phys = {}

    def add(self, eng, fn, reads=(), writes=(), dma_key=None, inc=16):
        op = Op(eng, fn, is_dma=dma_key is not None, key=dma_key)
        if dma_key is not None:
            if dma_key not in self.key_phys:
                self.key_phys[dma_key] = len(self.key_phys)
            dma_key = "q%d" % self.key_phys[dma_key]
            op.key = dma_key
            c = self.key_cnt.get(dma_key, 0) + inc
            self.key_cnt[dma_key] = c
            op.sig = ("d_" + dma_key, c)
            op.inc = inc
        deps = []
        for b in reads:
            deps.extend(self.last_writer.get(b, ()))
        for b in writes:
            deps.extend(self.last_writer.get(b, ()))
            deps.extend(self.readers.get(b, ()))
        if dma_key is not None:
            p = self.last_dma_on_key.get(dma_key)
            if p is not None:
                deps.append(p)
            self.last_dma_on_key[dma_key] = op
        if eng not in self.bar_applied:
            self.bar_applied.add(eng)
            deps.extend(self.bar)
        seen = set()
        for d in deps:
            if d is op or id(d) in seen:
                continue
            seen.add(id(d))
            if d.eng == eng and not d.is_dma:
                if not SAME_ENGINE_SYNC or eng == "pe":
                    continue
            op.deps.append(d)
            d.signal = True
        for b in writes:
            if self.readers.get(b):
                self.last_writer[b] = [op]
            else:
                self.last_writer.setdefault(b, []).append(op)
                if len(self.last_writer[b]) > 4:
                    self.last_writer[b] = self.last_writer[b][-4:]
            self.readers[b] = []
        for b in reads:
            self.readers.setdefault(b, []).append(op)
        self.ops[eng].append(op)
        self.nops += 1
        return op

    def emit(self, nc, final_wait_ops=()):
        for o in final_wait_ops:
            o.signal = True
        sem_names = []
        for e in ENGINES:
            cnt = 0
            gen = 0
            for op in self.ops[e]:
                if op.is_dma:
                    continue
                if op.signal:
                    if cnt >= SEM_ROLL:
                        gen += 1
                        cnt = 0
                    cnt += 1
                    name = "c_%s_%d" % (e, gen)
                    if name not in sem_names:
                        sem_names.append(name)
                    op.sig = (name, cnt)
        for e in ENGINES:
            for op in self.ops[e]:
                if op.is_dma and op.sig[0] not in sem_names:
                    sem_names.append(op.sig[0])
        with ExitStack() as es:
            sems = {n: es.enter_context(nc.semaphore(n)) for n in sem_names}
            block = es.enter_context(nc.Block())
            sched = self

            def run_engine(eng_name, eng):
                waited = {}
                for op in sched.ops[eng_name]:
                    for d in op.deps:
                        name, val = d.sig
                        if waited.get(name, 0) >= val:
                            continue
                        waited[name] = val
                        eng.wait_ge(sems[name], val)
                    ins = op.fn(eng)
                    if op.is_dma:
                        ins.then_inc(sems[op.sig[0]], op.inc) if op.inc != 1 else ins.then_inc(sems[op.sig[0]])
                    elif op.signal:
                        ins.then_inc(sems[op.sig[0]], 1)
                if eng_name == "sp":
                    for o in final_wait_ops:
                        name, val = o.sig
                        eng.wait_ge(sems[name], val)

            @block.tensor
            def _(e):
                run_engine("pe", e)

            @block.scalar
            def _(e):
                run_engine("act", e)

            @block.vector
            def _(e):
                run_engine("dve", e)

            @block.gpsimd
            def _(e):
                run_engine("pool", e)

            @block.sync
            def _(e):
                run_engine("sp", e)


class KB:
    def __init__(self):
        self.nc = bass.Bass("TRN2", target_bir_lowering=False)
        self.S = Sched()
        self.es = ExitStack()
        self.in_names = []
        self.out_names = []
        self.final_ops = []
        self.uid = 0
        self.psn = 0
        self.ext_in = set()
        self.ext_out = set()
        self.scr_out = []

    def din(self, name, shape, dt=F32):
        self.in_names.append(name)
        return self.nc.dram_tensor(name, list(shape), dt, kind="ExternalInput").ap()

    def dout(self, name, shape, dt=F32):
        self.out_names.append(name)
        return self.nc.dram_tensor(name, list(shape), dt, kind="ExternalOutput").ap()

    def dint(self, name, shape, dt=F32):
        if name in self.ext_in:
            return self.din(name, shape, dt)
        if name in self.ext_out:
            ap = self.dout(name, shape, dt)
            self.scr_out.append(name)
            return ap
        return self.nc.dram_tensor(name, list(shape), dt, kind="Internal").ap()

    def sb(self, name, shape, dt=F32):
        return self.es.enter_context(self.nc.sbuf_tensor(name, list(shape), dt))

    def psum(self, name, shape=(128, 512), dt=F32):
        return self.es.enter_context(self.nc.psum_tensor(name, list(shape), dt))

    def mm(self, out, lhsT, rhs, start, stop, r, w):
        return self.S.add("pe", lambda e: e.matmul(out, lhsT, rhs, start=start, stop=stop, skip_group_check=True), r, w)

    def act(self, out, in_, func, r, w, bias=None, scale=None, accum=None):
        kw = {}
        if bias is not None:
            kw["bias"] = bias
        if scale is not None:
            kw["scale"] = scale
        if accum is not None:
            kw["accum_out"] = accum
        return self.S.add("act", lambda e: e.activation(out=out, in_=in_, func=func, **kw), r, w)

    def ts(self, eng, out, in0, s1, s2, op0, op1, r, w, accum=None):
        kw = {}
        if accum is not None:
            kw["accum_out"] = accum
        if op1 is None:
            return self.S.add(eng, lambda e: e.tensor_scalar(out=out, in0=in0, scalar1=s1, scalar2=None, op0=op0, **kw), r, w)
        if s2 is None:
            return self.S.add(eng, lambda e: e.tensor_scalar(out=out, in0=in0, scalar1=s1, scalar2=None, op0=op0, op1=op1, **kw), r, w)
        return self.S.add(eng, lambda e: e.tensor_scalar(out=out, in0=in0, scalar1=s1, scalar2=s2, op0=op0, op1=op1, **kw), r, w)

    def tt(self, eng, out, in0, in1, op, r, w):
        return self.S.add(eng, lambda e: e.tensor_tensor(out=out, in0=in0, in1=in1, op=op), r, w)

    def stt(self, out, in0, scalar, in1, op0, op1, r, w):
        return self.S.add("dve", lambda e: e.scalar_tensor_tensor(out=out, in0=in0, scalar=scalar, in1=in1, op0=op0, op1=op1), r, w)

    def copy(self, eng, out, in_, r, w):
        if eng == "act":
            return self.S.add("act", lambda e: e.activation(out=out, in_=in_, func=AF.Copy), r, w)
        return self.S.add(eng, lambda e: e.tensor_copy(out=out, in_=in_), r, w)

    def memset(self, eng, ap, val, w):
        return self.S.add(eng, lambda e: e.memset(ap, val), (), w)

    def dma(self, q, out, in_, r, w, key):
        return self.S.add(q, lambda e: e.dma_start(out=out, in_=in_), r, w, dma_key=key)

    def finish(self):
        self.S.emit(self.nc, final_wait_ops=self.final_ops)
        self.es.close()
        return self.nc

    def push(self):
        ph = Phase(self)
        return ph

    def pop(self, ph):
        self.S.barrier()
        ph.es.close()


class Phase:
    def __init__(self, kb):
        self.kb = kb
        self.es = ExitStack()

    def sb(self, name, shape, dt=F32):
        return self.es.enter_context(self.kb.nc.sbuf_tensor(name, list(shape), dt))


class G:
    pass


def r3(ap):
    return ap.rearrange("p (h t) -> p h t", t=128)


def fm(ap):
    return ap.rearrange("(k p) t -> p k t", p=128)


def phase_mod(kb, g):
    ph = kb.push()
    c_sb = ph.sb("c_sb", [128, 8])
    cond = ph.sb("cond", [128, 8], BF16)
    brow = ph.sb("brow", [1, 6144])
    mrow = ph.sb("mrow", [1, 6144])
    ablk = [ph.sb("ablk%d" % i, [128, 8, 512], BF16) for i in range(2)]
    one11 = ph.sb("one11", [1, 2])
    kb.memset("dve", one11[:], 1.0, ["one11"])
    kb.dma("sp", c_sb[:], g.c, [], ["c_sb"], "c_sb")
    kb.act(cond[:], c_sb[:], AF.Silu, ["c_sb"], ["cond"])
    n = 0
    for l in range(2):
        kb.dma("sp", brow[:], g.ada_b[l], [], ["brow"], "brow")
        wv = g.ada_w[l].rearrange("(p k) n -> p k n", k=8)
        for j in range(12):
            slot = n % 2
            bk = "bk%d" % (n % 2)
            ps = g.bank[n % 2]
            n += 1
            kb.dma("pool", ablk[slot][:], wv[:, :, j * 512:(j + 1) * 512], [], ["ablk%d" % slot], "ablk%d" % slot)
            for k in range(8):
                kb.mm(ps[0:1, :], cond[:, k:k + 1], ablk[slot][:, k, :], k == 0, k == 7, ["cond", "ablk%d" % slot], [bk])
            kb.tt("dve", mrow[0:1, j * 512:(j + 1) * 512], ps[0:1, :], brow[0:1, j * 512:(j + 1) * 512], ALU.add,
                  [bk, "brow"], ["mrow"])
        pst = g.bank[2]
        for j in range(48):
            kb.mm(pst[:, j:j + 1], mrow[0:1, j * 128:(j + 1) * 128], one11[0:1, 0:1], True, True, ["mrow", "one11"], ["bk2"])
        kb.copy("act", g.modT[l][:], pst[:, 0:48], ["bk2"], ["modT%d" % l])
        kb.ts("dve", g.mod1[l][:], g.modT[l][:], 1.0, None, ALU.add, None, ["modT%d" % l], ["mod1%d" % l])
    kb.pop(ph)


def phase_A(kb, g):
    ph = kb.push()
    xa = [ph.sb("xa%d" % i, [128, 8, 512]) for i in range(2)]
    hT = [ph.sb("hT%d" % i, [128, 8, 512], BF16) for i in range(2)]
    slab = [ph.sb("slab%d" % i, [128, 8, 512], BF16) for i in range(2)]
    qst = [ph.sb("qst%d" % i, [64, 8, 512], BF16) for i in range(2)]
    vst = [ph.sb("vst%d" % i, [128, 8, 128], BF16) for i in range(2)]
    bgs = ph.sb("bgs", [128, 4, 512], BF16)
    cgs = ph.sb("cgs", [128, 4, 512])
    pext = ph.sb("pext", [128, 4, 514])
    ycv = ph.sb("ycv", [128, 512])
    ybst = [ph.sb("ybst%d" % i, [128, 512], BF16) for i in range(2)]
    kist = ph.sb("kist", [64, 512], BF16)
    wst = ph.sb("wst", [128, 4, 8])
    cw = ph.sb("cw", [128, 4, 3])
    kb.dma("sp", cw[:], g.ev_conv_w, [], ["cw"], "cw")
    kb.memset("dve", pext[:], 0.0, ["pext"])
    for i in range(2):
        kb.memset("pool", vst[i][:], 1.0, ["vst%d" % i])
    xv = fm(g.xT)
    wv = fm(g.ev_w_in)
    sc = g.mod1[0]
    sh = g.modT[0]
    nb = [0]
    ns = [0]
    nq = [0]
    nv = [0]
    ny = [0]

    def bank():
        i = nb[0] % 8
        nb[0] += 1
        return g.bank[i], "bk%d" % i

    for ti, (s, n) in enumerate(TT):
        sx = ti % 2
        nq128 = n // 128
        kb.dma("sp", xa[sx][:, :, 0:n], xv[:, :, s:s + n], [], ["xa%d" % sx], "xa%d" % sx)
        for kc in range(8):
            kb.act(hT[sx][:, kc, 0:n], xa[sx][:, kc, 0:n], AF.Identity, ["xa%d" % sx, "mod10", "modT0"], ["hT%d" % sx],
                   bias=sh[:, kc:kc + 1], scale=sc[:, 8 + kc:9 + kc])
        hk = "hT%d" % sx
        for blk in range(8):
            if blk not in A_BLKS:
                continue
            ss = ns[0] % 2
            ns[0] += 1
            ncol = 512 if blk < 7 else 72
            sk = "slab%d" % ss
            kb.dma("pool", slab[ss][:, :, 0:ncol], wv[:, :, blk * 512:blk * 512 + ncol], [], [sk], sk)
            if blk in (0, 1, 3):
                qs = nq[0] % 2
                nq[0] += 1
                qk = "qst%d" % qs
                for h in range(8):
                    ps, bk = bank()
                    for kc in range(8):
                        kb.mm(ps[0:64, 0:n], slab[ss][:, kc, h * 64:(h + 1) * 64], hT[sx][:, kc, 0:n], kc == 0, kc == 7,
                              [sk, hk], [bk])
                    kb.act(qst[qs][:, h, 0:n], ps[0:64, 0:n], AF.Copy, [bk], [qk], scale=(0.125 if blk == 0 else 1.0))
                dst = {0: g.qloc, 1: g.kloc, 3: g.qiloc}[blk]
                q0 = s // 128
                for sub in range(nq128):
                    kb.dma("sp", dst[q0 + sub], qst[qs][:, :, sub * 128:(sub + 1) * 128], [qk], [], "qout%d_%d" % (qs, sub))
                    if blk == 1 and q0 + sub >= 1:
                        kb.dma("sp", g.kown[q0 + sub - 1], qst[qs][:, :, sub * 128:(sub + 1) * 128], [qk], [], "qout%d_%d" % (qs, sub))
            elif blk == 2:
                for sub in range(nq128):
                    vs = nv[0] % 2
                    nv[0] += 1
                    vk = "vst%d" % vs
                    ps, bk = bank()
                    for kc in range(8):
                        kb.mm(ps[:, 0:512], hT[sx][:, kc, sub * 128:(sub + 1) * 128], slab[ss][:, kc, 0:512], kc == 0, kc == 7,
                              [sk, hk], [bk])
                    pv = ps[:, 0:512].rearrange("p (h d) -> p h d", d=64)
                    kb.copy("act", vst[vs][:, 0::2, 0:64], pv[:, 0::2, :], [bk], [vk])
                    kb.copy("dve", vst[vs][:, 1::2, 64:128], pv[:, 1::2, :], [bk], [vk])
                    kt = s // 128 + sub
                    kb.dma("sp", g.vloc[kt], vst[vs][:].rearrange("p h d -> p (h d)"), [vk], [], "vout%d" % vs)
                    if kt >= 1:
                        kb.dma("sp", g.vown[kt - 1], vst[vs][:].rearrange("p h d -> p (h d)"), [vk], [], "vout%d" % vs)
            elif blk in (4, 5, 6):
                for c in range(4):
                    ps, bk = bank()
                    for kc in range(8):
                        kb.mm(ps[:, 0:n], slab[ss][:, kc, c * 128:(c + 1) * 128], hT[sx][:, kc, 0:n], kc == 0, kc == 7,
                              [sk, hk], [bk])
                    if blk == 4:
                        kb.copy("act", bgs[:, c, 0:n], ps[:, 0:n], [bk], ["bgs"])
                    elif blk == 5:
                        kb.copy("act", cgs[:, c, 0:n], ps[:, 0:n], [bk], ["cgs"])
                    else:
                        pk = "pext%d" % c
                        kb.tt("dve", pext[:, c, 2:2 + n], ps[:, 0:n], cgs[:, c, 0:n], ALU.mult, [bk, "cgs", "pext"], [pk])
                        if ti == 0:
                            kb.ts("dve", pext[:, c, 2:2 + n], pext[:, c, 2:2 + n], g.hv[:, 0:1], None, ALU.mult, None, [pk, "hv"], [pk])
                        kb.ts("dve", ycv[:, 0:n], pext[:, c, 0:n], cw[:, c, 0:1], None, ALU.mult, None, [pk, "cw"], ["ycv"])
                        kb.stt(ycv[:, 0:n], pext[:, c, 1:1 + n], cw[:, c, 1:2], ycv[:, 0:n], ALU.mult, ALU.add, [pk, "cw", "ycv"], ["ycv"])
                        kb.stt(ycv[:, 0:n], pext[:, c, 2:2 + n], cw[:, c, 2:3], ycv[:, 0:n], ALU.mult, ALU.add, [pk, "cw", "ycv"], ["ycv"])
                        ys = ny[0] % 2
                        ny[0] += 1
                        yk = "ybst%d" % ys
                        kb.tt("dve", ybst[ys][:, 0:n], ycv[:, 0:n], bgs[:, c, 0:n], ALU.mult, ["ycv", "bgs"], [yk])
                        kb.dma("sp", g.ybloc[:, c, s:s + n], ybst[ys][:, 0:n], [yk], [], "ybout%d" % ys)
                        kb.copy("pool", pext[:, c, 0:2], pext[:, c, n:n + 2], [pk], [pk])
            else:
                ps, bk = bank()
                for kc in range(8):
                    kb.mm(ps[0:64, 0:n], slab[ss][:, kc, 0:64], hT[sx][:, kc, 0:n], kc == 0, kc == 7, [sk, hk], [bk])
                kb.copy("act", kist[:, 0:n], ps[0:64, 0:n], [bk], ["kist"])
                kb.dma("sp", g.kiloc[:, s:s + n], kist[:, 0:n], ["kist"], [], "kiout")
                if s >= 128:
                    kb.dma("sp", g.kiown[:, s - 128:s - 128 + n], kist[:, 0:n], ["kist"], [], "kiout")
                ps, bk = bank()
                for sub in range(nq128):
                    for kc in range(8):
                        kb.mm(ps[:, sub * 8:(sub + 1) * 8], hT[sx][:, kc, sub * 128:(sub + 1) * 128], slab[ss][:, kc, 64:72],
                              (kc == 0 and sub == 0), kc == 7, [sk, hk], [bk])
                kb.copy("act", wst[:, 0:nq128, :], ps[:, 0:nq128 * 8].rearrange("p (q e) -> p q e", e=8), [bk], ["wst"])
                q0 = s // 128
                kb.dma("sp", g.wloc[q0:q0 + nq128].rearrange("q p e -> p q e"), wst[:, 0:nq128, :], ["wst"], [], "wout")
    kb.pop(ph)


def allgather(kb, src2d, dst2d, rkey, wkey, key):
    return kb.S.add("pool", lambda e: e.collective_compute("AllGather", ALU.bypass, replica_groups=[list(range(NCORES))],
                                                           ins=[src2d], outs=[dst2d]), [rkey], [wkey], dma_key=key, inc=1)


def phase_B(kb, g, qtiles=None):
    ph = kb.push()
    W0 = SEQ + 128
    sc = ph.sb("sc", [128, W0 + OWN])
    selb = ph.sb("selb", [128, W0 + OWN], BF16)
    qa = ph.sb("qa", [68, 8, 128], BF16)
    qia = ph.sb("qia", [64, 8, 128], BF16)
    wia = ph.sb("wia", [128, 8])
    diag = ph.sb("diag", [128, 8, 128], BF16)
    kib = [ph.sb("kib%d" % i, [64, 512], BF16) for i in range(2)]
    mrb = [ph.sb("mrb%d" % i, [1, 512], BF16) for i in range(2)]
    rl = [ph.sb("rl%d" % i, [128, 512], BF16) for i in range(4)]
    ka = [ph.sb("ka%d" % i, [68, 8, 128], BF16) for i in range(3)]
    va = [ph.sb("va%d" % i, [128, 8, 128], BF16) for i in range(3)]
    pT = [ph.sb("pT%d" % i, [128, 512], BF16) for i in range(3)]
    identb = ph.sb("identb_s", [128, 128], BF16)
    psw = ph.sb("psw_s", [128, 128])
    dtab = ph.sb("dtab_s", [128, 8, 128], BF16)
    onesr = ph.sb("onesr", [1, 128], BF16)
    fh = ph.sb("fh", [1, 128], BF16)
    shm = ph.sb("shm", [1, 128], BF16)
    rec = ph.sb("rec", [128, 512])
    rs = ph.sb("rs", [128, 512])
    yst = ph.sb("yst", [128, 2, 128], BF16)
    sm = {n: ph.sb("bs_" + n, [128, 1]) for n in ("lo", "hi", "mid", "cnt", "p", "d", "negm", "sa")}
    kb.dma("sp", identb[:], g.identb, [], ["identb"], "cst0")
    kb.dma("sp", psw[:], g.psw, [], ["psw"], "cst1")
    kb.dma("sp", dtab[:], g.dtab, [], ["dtab"], "cst2")
    kb.memset("dve", onesr[:], 1.0, ["onesr"])
    kb.memset("dve", fh[:], 0.0, ["fh"])
    kb.memset("dve", fh[0:1, 0:64], 1.0, ["fh"])
    kb.memset("dve", shm[:], 0.0, ["shm"])
    kb.memset("dve", shm[0:1, 64:128], -BIGI, ["shm"])
    kb.memset("dve", rec[:], 0.0, ["rec"])
    nrel = [0]
    nacc = [0]
    nkb = [0]
    nka = [0]
    npt = [0]
    kig = g.kig
    for i in (range(NQT) if qtiles is None else qtiles):
        kb.dma("sp", qa[0:64], g.qloc[i], [], ["qa"], "qa")
        kb.dma("sp", qa[64:68], r3(g.qcst[i]), [], ["qa"], "qac")
        kb.dma("sp", qia[:], g.qiloc[i], [], ["qia"], "qia")
        kb.dma("sp", wia[:], g.wloc[i], [], ["wia"], "wia")
        for h in range(8):
            kb.ts("pool", diag[:, h, :], identb[:], wia[:, h:h + 1], None, ALU.mult, None, ["identb", "wia"], ["diag"])
        W = W0 + 128 * i
        blocks = []
        for b in range(32):
            cr, cb = b // 4, b % 4
            blocks.append((kig[cr * 64:(cr + 1) * 64, cb * 512:(cb + 1) * 512], g.mrowg[0:1, b * 512:(b + 1) * 512], b * 512, 512, None))
        blocks.append((g.kiloc[:, 0:128], g.mrowl[0:1, 0:128], SEQ, 128, (0 if i == 0 else None)))
        nown = 128 * i
        o = 0
        while o < nown:
            nk = min(512, nown - o)
            dg = None
            if o + nk == nown:
                dg = nk - 128
            blocks.append((g.kiloc[:, 128 + o:128 + o + nk], g.mrowl[0:1, 128 + o:128 + o + nk], W0 + o, nk, dg))
            o += nk
        for (src, msk, off, nk, dg) in blocks:
            ks = nkb[0] % 2
            nkb[0] += 1
            kk, mk = "kib%d" % ks, "mrb%d" % ks
            kb.dma("sp", kib[ks][:, 0:nk], src, [], [kk], kk)
            kb.dma("sp", mrb[ks][0:1, 0:nk], msk, [], [mk], mk)
            ai = 4 + nacc[0] % 2
            nacc[0] += 1
            acc, ak = g.bank[ai], "bk%d" % ai
            pend = []
            for h in range(8):
                ri = nrel[0] % 4
                rsl = nrel[0] % 4
                nrel[0] += 1
                rel, rk = g.bank[ri], "bk%d" % ri
                kb.mm(rel[:, 0:nk], qia[:, h, :], kib[ks][:, 0:nk], True, True, ["qia", kk], [rk])
                kb.act(rl[rsl][:, 0:nk], rel[:, 0:nk], AF.Relu, [rk], ["rl%d" % rsl])
                pend.append((h, rsl))
                if len(pend) > 2:
                    h_, r_ = pend.pop(0)
                    kb.mm(acc[:, 0:nk], diag[:, h_, :], rl[r_][:, 0:nk], h_ == 0, False, ["diag", "rl%d" % r_], [ak])
            for (h_, r_) in pend:
                kb.mm(acc[:, 0:nk], diag[:, h_, :], rl[r_][:, 0:nk], h_ == 0, False, ["diag", "rl%d" % r_], [ak])
            kb.mm(acc[:, 0:nk], onesr[0:1, :], mrb[ks][0:1, 0:nk], False, dg is None, ["onesr", mk], [ak])
            if dg is not None:
                kb.mm(acc[:, dg:dg + 128], fh[0:1, :], shm[0:1, :], False, True, ["fh", "shm"], [ak])
            kb.copy("dve", sc[:, off:off + nk], acc[:, 0:nk], [ak], ["sc"])
        lo, hi, mid, cnt, pp, dd = (sm[n] for n in ("lo", "hi", "mid", "cnt", "p", "d"))
        negm, sa = sm["negm"], sm["sa"]
        kb.S.add("dve", lambda e, W=W: e.tensor_reduce(out=hi[:], in_=sc[:, 0:W], axis=mybir.AxisListType.X, op=ALU.max), ["sc"], ["bs_hi"])
        kb.memset("dve", lo[:], -1024.0, ["bs_lo"])
        Wd = (int(W * 0.42) // 128) * 128
        nA = W - Wd
        for it in range(NBIS):
            kb.ts("dve", mid[:], lo[:], hi[:, 0:1], 0.5, ALU.add, ALU.mult, ["bs_lo", "bs_hi"], ["bs_mid"])
            kb.ts("dve", negm[:], mid[:], -1.0, None, ALU.mult, None, ["bs_mid"], ["bs_negm"])
            kb.act(selb[:, Wd:W], sc[:, Wd:W], AF.Sign, ["sc", "bs_negm"], (["selb", "selbA", "bs_sa"] if it == 0 else ["selbA", "bs_sa"]),
                   bias=negm[:, 0:1], scale=1.0, accum=sa[:])
            kb.ts("dve", selb[:, 0:Wd], sc[:, 0:Wd], mid[:, 0:1], None, ALU.is_gt, ALU.add, ["sc", "bs_mid"],
                  (["selb", "selbD", "bs_cnt"] if it == 0 else ["selbD", "bs_cnt"]), accum=cnt[:])
            kb.stt(cnt[:], sa[:], 0.5, cnt[:], ALU.mult, ALU.add, ["bs_sa", "bs_cnt"], ["bs_cnt"])
            kb.ts("dve", pp[:], cnt[:], 255.5 - nA / 2.0, None, ALU.is_gt, None, ["bs_cnt"], ["bs_p"])
            kb.tt("dve", dd[:], mid[:], lo[:], ALU.subtract, ["bs_mid", "bs_lo"], ["bs_d"])
            kb.stt(lo[:], dd[:], pp[:, 0:1], lo[:], ALU.mult, ALU.add, ["bs_d", "bs_p", "bs_lo"], ["bs_lo"])
            kb.tt("dve", dd[:], hi[:], mid[:], ALU.subtract, ["bs_hi", "bs_mid"], ["bs_d"])
            kb.stt(hi[:], dd[:], pp[:, 0:1], mid[:], ALU.mult, ALU.add, ["bs_d", "bs_p", "bs_mid"], ["bs_hi"])
        kb.ts("dve", selb[:, 0:W], sc[:, 0:W], lo[:, 0:1], -BIGS, ALU.is_le, ALU.mult, ["sc", "bs_lo"], ["selb", "selbA", "selbD"])
        if getattr(g, "dbgB", False):
            o1 = kb.dout("dbg_sc", [128, W0 + OWN])
            o2 = kb.dout("dbg_lo", [128, 1])
            o3 = kb.dout("dbg_cnt", [128, 1])
            kb.final_ops.append(kb.dma("sp", o1, sc[:], ["sc"], [], "dbgsc"))
            kb.final_ops.append(kb.dma("sp", o2, lo[:], ["bs_lo"], [], "dbglo"))
            kb.final_ops.append(kb.dma("sp", o3, cnt[:], ["bs_cnt"], [], "dbgcnt"))
        tiles = []
        for kt in range(128):
            tiles.append((g.kg[kt * 64:(kt + 1) * 64, :], g.kcg[kt], g.vg[kt * 128:(kt + 1) * 128, :], kt * 128, False))
        for j in range(i + 1):
            col = SEQ if j == 0 else W0 + 128 * (j - 1)
            tiles.append((g.kloc[j], g.kcl[j], g.vloc[j], col, j == i))
        nt = len(tiles)
        pvq = []
        for ti, (ksrc, csrc, vsrc, col, isdiag) in enumerate(tiles):
            sl = nka[0] % 3
            nka[0] += 1
            kk, vk = "ka%d" % sl, "va%d" % sl
            kb.dma("sp", ka[sl][0:64], (r3(ksrc) if len(ksrc.shape) == 2 else ksrc), [], [kk], kk)
            kb.dma("sp", ka[sl][64:68], r3(csrc), [], [kk], kk + "c")
            kb.dma("sp", va[sl][:], r3(vsrc), [], [vk], vk)
            for hg in range(2):
                si = nrel[0] % 4
                nrel[0] += 1
                S_, sk = g.bank[si], "bk%d" % si
                for h4 in range(4):
                    h = hg * 4 + h4
                    cs = slice(h4 * 128, (h4 + 1) * 128)
                    kb.mm(S_[:, cs], ka[sl][0:68, h, :], qa[0:68, h, :], h4 == 0, False, [kk, "qa"], [sk])
                    if isdiag:
                        kb.mm(S_[:, cs], identb[:], dtab[:, h, :], False, False, ["identb", "dtab"], [sk])
                    kb.mm(S_[:, cs], selb[:, col:col + 128], identb[:], False, True, ["selb", "identb"], [sk])
                ps_ = npt[0] % 3
                npt[0] += 1
                pk = "pT%d" % ps_
                kb.act(pT[ps_][:], S_[:, 0:512], AF.Exp, [sk], [pk])
                def pv(hg=hg, sl=sl, ps_=ps_, vk=vk, pk=pk, ti=ti):
                    accb, ak = g.bank[6 + hg], "bk%d" % (6 + hg)
                    for h4 in range(4):
                        h = hg * 4 + h4
                        cs = slice(h4 * 128, (h4 + 1) * 128)
                        kb.mm(accb[:, cs], va[sl][:, h, :], pT[ps_][:, cs], (ti == 0 and h4 == 0), ti == nt - 1, [vk, pk], [ak])
                pvq.append(pv)
                if len(pvq) > 2:
                    pvq.pop(0)()
        while pvq:
            pvq.pop(0)()
        for hg in range(2):
            accb, ak = g.bank[6 + hg], "bk%d" % (6 + hg)
            av = accb[:, 0:512].rearrange("p (h t) -> p h t", t=128)
            rv = rec[:].rearrange("p (h t) -> p h t", t=128)
            sv = rs[:].rearrange("p (h t) -> p h t", t=128)
            kb.ts("dve", rv[64:128, 0::2, :], av[64:128, 0::2, :], 1e-20, None, ALU.add, None, [ak], ["rec"])
            kb.ts("dve", rv[0:64, 1::2, :], av[0:64, 1::2, :], 1e-20, None, ALU.add, None, [ak], ["rec"])
            kb.S.add("dve", lambda e, rv=rv: e.reciprocal(out=rv[64:128, 0::2, :], in_=rv[64:128, 0::2, :]), ["rec"], ["rec"])
            kb.S.add("dve", lambda e, rv=rv: e.reciprocal(out=rv[0:64, 1::2, :], in_=rv[0:64, 1::2, :]), ["rec"], ["rec"])
            shb, shk = g.bank[4], "bk4"
            kb.mm(shb[:, 0:512], psw[:], rec[:], True, True, ["psw", "rec"], [shk])
            kb.copy("act", rs[:], shb[:, 0:512], [shk], ["rs"])
            kb.tt("dve", yst[0:64, :, :], av[0:64, 0::2, :], sv[0:64, 0::2, :], ALU.mult, [ak, "rs"], ["yst"])
            kb.tt("dve", yst[64:128, :, :], av[64:128, 1::2, :], sv[64:128, 1::2, :], ALU.mult, [ak, "rs"], ["yst"])
            kb.dma("sp", g.yaloc[:, hg * 2:(hg + 1) * 2, i * 128:(i + 1) * 128], yst[:], ["yst"], [], "yaout")
    kb.pop(ph)


class LNCtx:
    def __init__(self, kb, g, ph, tag):
        self.onesm = ph.sb(tag + "_onesm", [128, 128])
        self.sqt = [ph.sb(tag + "_sq%d" % i, [128, 512]) for i in range(2)]
        self.mean = ph.sb(tag + "_mean", [128, 512])
        self.var = ph.sb(tag + "_var", [128, 512])
        self.t1 = [ph.sb(tag + "_t1%d" % i, [128, 512]) for i in range(2)]
        self.tmp = [ph.sb(tag + "_tmp%d" % i, [128, 512]) for i in range(2)]
        self.wsl = [ph.sb(tag + "_wsl%d" % i, [128, 22, 128], BF16) for i in range(2)]
        self.tag = tag
        self.n = 0
        kb.memset("dve", self.onesm[:], 1.0 / D, [tag + "_onesm"])


def res_ln(kb, g, L, s, n, y_fn, gate_ap, x_fn, lng, lnb, rkeys):
    tag = L.tag
    xres = g.xres
    for m in range(8):
        yps, yk = y_fn(m)
        xa_, xk = x_fn(m)
        tp = L.n % 2
        L.n += 1
        tk = tag + "_tmp%d" % tp
        kb.ts("dve", L.tmp[tp][:, 0:n], yps, gate_ap[:, m:m + 1], None, ALU.mult, None, [yk] + rkeys, [tk])
        kb.stt(xres[:, m, s:s + n], xa_, DN_ALPHA, L.tmp[tp][:, 0:n], ALU.mult, ALU.add, [xk, tk], ["xres"])
    mb, mk = g.bank[6], "bk6"
    qb, qk = g.bank[7], "bk7"
    for m in range(8):
        sp_ = m % 2
        sk = tag + "_sq%d" % sp_
        kb.act(L.sqt[sp_][:, 0:n], xres[:, m, s:s + n], AF.Square, ["xres"], [sk])
        kb.mm(mb[:, 0:n], L.onesm[:], xres[:, m, s:s + n], m == 0, m == 7, [tag + "_onesm", "xres"], [mk])
        kb.mm(qb[:, 0:n], L.onesm[:], L.sqt[sp_][:, 0:n], m == 0, m == 7, [tag + "_onesm", sk], [qk])
    kb.copy("act", L.mean[:, 0:n], mb[:, 0:n], [mk], [tag + "_mean"])
    kb.tt("dve", L.var[:, 0:n], L.mean[:, 0:n], L.mean[:, 0:n], ALU.mult, [tag + "_mean"], [tag + "_var"])
    kb.tt("dve", L.var[:, 0:n], qb[:, 0:n], L.var[:, 0:n], ALU.subtract, [qk, tag + "_var"], [tag + "_var"])
    kb.ts("dve", L.var[:, 0:n], L.var[:, 0:n], LN_EPS, None, ALU.add, None, [tag + "_var"], [tag + "_var"])
    kb.act(L.var[:, 0:n], L.var[:, 0:n], AF.Sqrt, [tag + "_var"], [tag + "_var"])
    kb.S.add("dve", lambda e: e.reciprocal(out=L.var[:, 0:n], in_=L.var[:, 0:n]), [tag + "_var"], [tag + "_var"])
    for m in range(8):
        tp = m % 2
        tk = tag + "_t1%d" % tp
        kb.tt("dve", L.t1[tp][:, 0:n], xres[:, m, s:s + n], L.mean[:, 0:n], ALU.subtract, ["xres", tag + "_mean"], [tk])
        kb.tt("dve", L.t1[tp][:, 0:n], L.t1[tp][:, 0:n], L.var[:, 0:n], ALU.mult, [tk, tag + "_var"], [tk])
        kb.act(xres[:, m, s:s + n], L.t1[tp][:, 0:n], AF.Identity, [tk] + rkeys, ["xres"], bias=lnb[:, m:m + 1], scale=lng[:, m:m + 1])


def out_proj(kb, g, L, w_dram, KC, rhs_fn, s, n, nbank):
    wv = w_dram.rearrange("(k p) m -> p k m", p=128)

    def y_fn(m):
        sl = nbank[0] % 2
        bi = nbank[0] % 4
        nbank[0] += 1
        wk = L.tag + "_wsl%d" % sl
        kb.dma("pool", L.wsl[sl][:, 0:KC, :], wv[:, :, m * 128:(m + 1) * 128], [], [wk], wk)
        ps, bk = g.bank[bi], "bk%d" % bi
        for kc in range(KC):
            ra, rk = rhs_fn(kc)
            kb.mm(ps[:, 0:n], L.wsl[sl][:, kc, :], ra, kc == 0, kc == KC - 1, [wk, rk], [bk])
        return ps[:, 0:n], bk
    return y_fn


def phase_B2(kb, g, l=0):
    ph = kb.push()
    L = LNCtx(kb, g, ph, "b2%d" % l)
    xa = [ph.sb("b2%d_xa%d" % (l, i), [128, 8, 512]) for i in range(2)] if l == 0 else None
    cat = [ph.sb("b2%d_cat%d" % (l, i), [128, 8, 512], BF16) for i in range(2)]
    xv = fm(g.xT)
    nbank = [0]
    srcA, srcB, wout = (g.yaloc, g.ybloc, g.ev_w_out) if l == 0 else (g.ycloc, g.ydloc, g.od_w_out)
    for ti, (s, n) in enumerate(TT):
        sx = ti % 2
        xk, ck = "b2%d_xa%d" % (l, sx), "b2%d_cat%d" % (l, sx)
        if l == 0:
            kb.dma("sp", xa[sx][:, :, 0:n], xv[:, :, s:s + n], [], [xk], xk)
        kb.dma("sp", cat[sx][:, 0:4, 0:n], srcA[:, :, s:s + n], [], [ck], ck + "a")
        kb.dma("sp", cat[sx][:, 4:8, 0:n], srcB[:, :, s:s + n], [], [ck], ck + "b")
        y_fn = out_proj(kb, g, L, wout, 8, lambda kc: (cat[sx][:, kc, 0:n], ck), s, n, nbank)
        if l == 0:
            x_fn = lambda m: (xa[sx][:, m, 0:n], xk)
        else:
            x_fn = lambda m: (g.xres[:, m, s:s + n], "xres")
        res_ln(kb, g, L, s, n, y_fn, g.mod1[l][:, 16:24], x_fn, g.lnp["mix%dg" % l], g.lnp["mix%db" % l],
               ["mod1%d" % l, "lnp"])
    kb.pop(ph)


def phase_FFN(kb, g, l):
    ph = kb.push()
    tag = "f%d" % l
    L = LNCtx(kb, g, ph, tag)
    hT = [ph.sb(tag + "_hT%d" % i, [128, 8, 512], BF16) for i in range(2)]
    wup = [ph.sb(tag + "_wup%d" % i, [128, 8, 256], BF16) for i in range(2)]
    uext = [ph.sb(tag + "_ue%d" % i, [128, 514]) for i in range(4)]
    carry = ph.sb(tag + "_carry", [128, 44, 2])
    cv = [ph.sb(tag + "_cv%d" % i, [128, 512]) for i in range(2)]
    sg = ph.sb(tag + "_sg", [128, 512])
    actT = ph.sb(tag + "_actT", [128, NPAIR, 512], BF16)
    cw = ph.sb(tag + "_cw", [128, 44, 3])
    kb.dma("sp", cw[:], g.ffn_conv_w[l], [], [tag + "_cw"], tag + "_cw")
    kb.memset("dve", carry[:], 0.0, [tag + "_carry"])
    wv = fm(g.ffn_w_up[l])
    sc, sh = g.mod1[l], g.modT[l]
    mk = ["mod1%d" % l, "modT%d" % l]
    nw = [0]
    nu = [0]
    nbank = [0]
    ck_ = tag + "_carry"
    for ti, (s, n) in enumerate(TT):
        sx = ti % 2
        hk = tag + "_hT%d" % sx
        for kc in range(8):
            kb.act(hT[sx][:, kc, 0:n], g.xres[:, kc, s:s + n], AF.Identity, ["xres"] + mk, [hk],
                   bias=sh[:, 24 + kc:25 + kc], scale=sc[:, 32 + kc:33 + kc])
        for j in range(NPAIR):
            ws = nw[0] % 2
            nw[0] += 1
            wk = tag + "_wup%d" % ws
            kb.dma("pool", wup[ws][:, :, 0:128], wv[:, :, j * 128:(j + 1) * 128], [], [wk], wk + "v")
            kb.dma("pool", wup[ws][:, :, 128:256], wv[:, :, DFF + j * 128:DFF + (j + 1) * 128], [], [wk], wk + "g")
            res = []
            for half in range(2):
                bi = nbank[0] % 4
                nbank[0] += 1
                ps, bk = g.bank[bi], "bk%d" % bi
                for kc in range(8):
                    kb.mm(ps[:, 0:n], wup[ws][:, kc, half * 128:(half + 1) * 128], hT[sx][:, kc, 0:n], kc == 0, kc == 7, [wk, hk], [bk])
                ui = nu[0] % 4
                nu[0] += 1
                uk = tag + "_ue%d" % ui
                ch = j + half * NPAIR
                kb.copy("pool", uext[ui][:, 0:2], carry[:, ch, :], [ck_], [uk])
                kb.copy("act", uext[ui][:, 2:2 + n], ps[:, 0:n], [bk], [uk])
                kb.copy("pool", carry[:, ch, :], uext[ui][:, n:n + 2], [uk], [ck_])
                if ti == 0:
                    kb.ts("pool", carry[:, ch, :], carry[:, ch, :], g.hv[:, 0:1], None, ALU.mult, None, [ck_, "hv"], [ck_])
                ci = half
                cvk = tag + "_cv%d" % ci
                kb.ts("dve", cv[ci][:, 0:n], uext[ui][:, 0:n], cw[:, ch, 0:1], None, ALU.mult, None, [uk, tag + "_cw"], [cvk])
                kb.stt(cv[ci][:, 0:n], uext[ui][:, 1:1 + n], cw[:, ch, 1:2], cv[ci][:, 0:n], ALU.mult, ALU.add, [uk, cvk, tag + "_cw"], [cvk])
                kb.stt(cv[ci][:, 0:n], uext[ui][:, 2:2 + n], cw[:, ch, 2:3], cv[ci][:, 0:n], ALU.mult, ALU.add, [uk, cvk, tag + "_cw"], [cvk])
            kb.act(sg[:, 0:n], cv[1][:, 0:n], AF.Silu, [tag + "_cv1"], [tag + "_sg"])
            kb.tt("dve", actT[:, j, 0:n], sg[:, 0:n], cv[0][:, 0:n], ALU.mult, [tag + "_sg", tag + "_cv0"], [tag + "_actT"])
        y_fn = out_proj(kb, g, L, g.ffn_w_down[l], NPAIR, lambda kc: (actT[:, kc, 0:n], tag + "_actT"), s, n, nbank)
        res_ln(kb, g, L, s, n, y_fn, g.mod1[l][:, 40:48], lambda m: (g.xres[:, m, s:s + n], "xres"),
               g.lnp["ffn%dg" % l], g.lnp["ffn%db" % l], ["mod1%d" % l, "lnp"])
    kb.pop(ph)


def phase_C2(kb, g):
    ph = kb.push()
    hT = [ph.sb("c2_hT%d" % i, [128, 8, 512], BF16) for i in range(2)]
    slab = [ph.sb("c2_slab%d" % i, [128, 8, 512], BF16) for i in range(2)]
    ust = [ph.sb("c2_ust%d" % i, [128, 512]) for i in range(2)]
    lat = ph.sb("c2_lat", [128, 5, 512])
    latn = ph.sb("c2_latn", [128, 5, 512], BF16)
    sq = [ph.sb("c2_sq%d" % i, [128, 512]) for i in range(2)]
    rstd = ph.sb("c2_rstd", [128, 2, 512])
    ones3 = ph.sb("c2_ones3", [128, 128])
    ones2 = ph.sb("c2_ones2", [128, 128])
    wq = ph.sb("c2_wq", [128, 3, 1536], BF16)
    wkv = ph.sb("c2_wkv", [128, 2, 1024], BF16)
    cq = ph.sb("c2_cq", [96, 512])
    sq_ = ph.sb("c2_sqt", [96, 512])
    t1 = [ph.sb("c2_t1%d" % i, [96, 512]) for i in range(2)]
    t2 = [ph.sb("c2_t2%d" % i, [96, 512]) for i in range(2)]
    qst = [ph.sb("c2_qst%d" % i, [96, 8, 512], BF16) for i in range(2)]
    kst = ph.sb("c2_kst", [96, 8, 512], BF16)
    krot = ph.sb("c2_krot", [32, 512], BF16)
    vst = [ph.sb("c2_vst%d" % i, [128, 8, 128], BF16) for i in range(2)]
    gq = ph.sb("c2_gq", [128, 5])
    cosk_sb = ph.sb("c2_cosk", [32, LT])
    sink_sb = ph.sb("c2_sink", [32, LT])
    kb.dma("sp", cosk_sb[:], g.cosk, [], ["cosk"], "c2_cosk")
    kb.dma("sp", sink_sb[:], g.sink, [], ["sink"], "c2_sink")
    kb.dma("sp", gq[:], g.latg, [], ["c2_gq"], "c2_gq")
    kb.memset("dve", ones3[:], 1.0 / 384.0, ["c2_ones3"])
    kb.memset("dve", ones2[:], 1.0 / 256.0, ["c2_ones2"])
    for i in range(2):
        kb.memset("pool", vst[i][:], 1.0, ["c2_vst%d" % i])
    kb.dma("pool", wq[:], g.w_uq2.rearrange("(k p) m -> p k m", p=128), [], ["c2_wq"], "c2_wq")
    kb.dma("pool", wkv[:], g.w_ukv2.rearrange("(k p) m -> p k m", p=128), [], ["c2_wkv"], "c2_wkv")
    wv = fm(g.od_w_in)
    sc, sh = g.mod1[1], g.modT[1]
    nb = [0]
    ns = [0]
    nv = [0]

    def bank():
        i = nb[0] % 6
        nb[0] += 1
        return g.bank[i], "bk%d" % i

    for ti, (s, n) in enumerate(TT):
        sx = ti % 2
        hk = "c2_hT%d" % sx
        nq128 = n // 128
        q0 = s // 128
        for kc in range(8):
            kb.act(hT[sx][:, kc, 0:n], g.xres[:, kc, s:s + n], AF.Identity, ["xres", "mod11", "modT1"], [hk],
                   bias=sh[:, kc:kc + 1], scale=sc[:, 8 + kc:9 + kc])
        kb.dma("sp", cq[:, 0:n], g.cosq[:, s:s + n], [], ["c2_cq"], "c2_cq")
        kb.dma("sp", sq_[:, 0:n], g.sinq[:, s:s + n], [], ["c2_sqt"], "c2_sqt")
        for blk in range(3):
            ss = ns[0] % 2
            ns[0] += 1
            sk = "c2_slab%d" % ss
            c0 = blk * 512
            ncol = 512 if blk < 2 else 192
            kb.dma("pool", slab[ss][:, :, 0:ncol], wv[:, :, c0:c0 + ncol], [], [sk], sk)
            if blk == 0:
                for c in range(4):
                    ps, bk = bank()
                    for kc in range(8):
                        kb.mm(ps[:, 0:n], slab[ss][:, kc, c * 128:(c + 1) * 128], hT[sx][:, kc, 0:n], kc == 0, kc == 7, [sk, hk], [bk])
                    us = c % 2
                    kb.copy("act", ust[us][:, 0:n], ps[:, 0:n], [bk], ["c2_ust%d" % us])
                    kb.dma("sp", g.uloc[:, c, s:s + n], ust[us][:, 0:n], ["c2_ust%d" % us], [], "c2_uout%d" % us)
            elif blk == 1:
                for c in range(4):
                    ps, bk = bank()
                    for kc in range(8):
                        kb.mm(ps[:, 0:n], slab[ss][:, kc, c * 128:(c + 1) * 128], hT[sx][:, kc, 0:n], kc == 0, kc == 7, [sk, hk], [bk])
                    kb.copy("act", lat[:, c, 0:n], ps[:, 0:n], [bk], ["c2_lat"])
            else:
                ps, bk = bank()
                for kc in range(8):
                    kb.mm(ps[:, 0:n], slab[ss][:, kc, 0:128], hT[sx][:, kc, 0:n], kc == 0, kc == 7, [sk, hk], [bk])
                kb.copy("act", lat[:, 4, 0:n], ps[:, 0:n], [bk], ["c2_lat"])
                pa, ak = bank()
                pb, bk2 = bank()
                for kc in range(8):
                    kb.mm(pa[0:32, 0:n], slab[ss][:, kc, 128:160], hT[sx][:, kc, 0:n], kc == 0, kc == 7, [sk, hk], [ak])
                for kc in range(8):
                    kb.mm(pb[0:32, 0:n], slab[ss][:, kc, 160:192], hT[sx][:, kc, 0:n], kc == 0, kc == 7, [sk, hk], [bk2])
                kb.tt("dve", t1[0][0:32, 0:n], pa[0:32, 0:n], cosk_sb[:, s:s + n], ALU.mult, [ak, "cosk"], ["c2_t10"])
                kb.tt("dve", t2[0][0:32, 0:n], pb[0:32, 0:n], sink_sb[:, s:s + n], ALU.mult, [bk2, "sink"], ["c2_t20"])
                kb.tt("dve", krot[:, 0:n], t1[0][0:32, 0:n], t2[0][0:32, 0:n], ALU.add, ["c2_t10", "c2_t20"], ["c2_krot"])
        for (c_lo, c_hi, onesm, ok, ri) in ((0, 3, ones3, "c2_ones3", 0), (3, 5, ones2, "c2_ones2", 1)):
            mb, mk = bank()
            for c in range(c_lo, c_hi):
                sp_ = c % 2
                kb.act(sq[sp_][:, 0:n], lat[:, c, 0:n], AF.Square, ["c2_lat"], ["c2_sq%d" % sp_])
                kb.mm(mb[:, 0:n], onesm[:], sq[sp_][:, 0:n], c == c_lo, c == c_hi - 1, [ok, "c2_sq%d" % sp_], [mk])
            kb.ts("dve", rstd[:, ri, 0:n], mb[:, 0:n], RMS_EPS, None, ALU.add, None, [mk], ["c2_rstd"])
            kb.act(rstd[:, ri, 0:n], rstd[:, ri, 0:n], AF.Sqrt, ["c2_rstd"], ["c2_rstd"])
            kb.S.add("dve", lambda e, ri=ri, n=n: e.reciprocal(out=rstd[:, ri, 0:n], in_=rstd[:, ri, 0:n]), ["c2_rstd"], ["c2_rstd"])
            for c in range(c_lo, c_hi):
                kb.stt(latn[:, c, 0:n], lat[:, c, 0:n], gq[:, c:c + 1], rstd[:, ri, 0:n], ALU.mult, ALU.mult,
                       ["c2_lat", "c2_gq", "c2_rstd"], ["c2_latn"])
        qs = ti % 2
        qk = "c2_qst%d" % qs
        for h in range(8):
            pa, ak = bank()
            pb, bk2 = bank()
            for kc in range(3):
                kb.mm(pa[0:96, 0:n], wq[:, kc, h * 96:(h + 1) * 96], latn[:, kc, 0:n], kc == 0, kc == 2, ["c2_wq", "c2_latn"], [ak])
            for kc in range(3):
                kb.mm(pb[0:96, 0:n], wq[:, kc, 768 + h * 96:768 + (h + 1) * 96], latn[:, kc, 0:n], kc == 0, kc == 2, ["c2_wq", "c2_latn"], [bk2])
            tp = h % 2
            kb.tt("dve", t1[tp][:, 0:n], pa[0:96, 0:n], cq[:, 0:n], ALU.mult, [ak, "c2_cq"], ["c2_t1%d" % tp])
            kb.tt("dve", t2[tp][:, 0:n], pb[0:96, 0:n], sq_[:, 0:n], ALU.mult, [bk2, "c2_sqt"], ["c2_t2%d" % tp])
            kb.tt("dve", qst[qs][:, h, 0:n], t1[tp][:, 0:n], t2[tp][:, 0:n], ALU.add, ["c2_t1%d" % tp, "c2_t2%d" % tp], [qk])
        for sub in range(nq128):
            kb.dma("sp", g.q1loc[q0 + sub], qst[qs][:, :, sub * 128:(sub + 1) * 128], [qk], [], "c2_qout%d" % sub)
        for h in range(8):
            pa, ak = bank()
            for kc in range(2):
                kb.mm(pa[0:64, 0:n], wkv[:, kc, h * 64:(h + 1) * 64], latn[:, 3 + kc, 0:n], kc == 0, kc == 1, ["c2_wkv", "c2_latn"], [ak])
            kb.copy("act", kst[0:64, h, 0:n], pa[0:64, 0:n], [ak], ["c2_kst"])
        for sub in range(nq128):
            cs = slice(sub * 128, (sub + 1) * 128)
            kb.dma("sp", g.k1loc[q0 + sub, 0:64], kst[0:64, :, cs], ["c2_kst"], [], "c2_kout%d" % sub)
            if q0 + sub >= 1:
                kb.dma("sp", g.k1own[q0 + sub - 1, 0:64], kst[0:64, :, cs], ["c2_kst"], [], "c2_kout%d" % sub)
            for h in range(8):
                kb.dma("sp", g.k1loc[q0 + sub, 64:96, h, :], krot[:, cs], ["c2_krot"], [], "c2_krout%d" % (h % 4))
                if q0 + sub >= 1:
                    kb.dma("sp", g.k1own[q0 + sub - 1, 64:96, h, :], krot[:, cs], ["c2_krot"], [], "c2_krout%d" % (h % 4))
        for sub in range(nq128):
            vs = nv[0] % 2
            nv[0] += 1
            vk = "c2_vst%d" % vs
            ps, bk = bank()
            for kc in range(2):
                kb.mm(ps[:, 0:512], latn[:, 3 + kc, sub * 128:(sub + 1) * 128], wkv[:, kc, 512:1024], kc == 0, kc == 1, ["c2_latn", "c2_wkv"], [bk])
            pv = ps[:, 0:512].rearrange("p (h d) -> p h d", d=64)
            kb.copy("act", vst[vs][:, 0::2, 0:64], pv[:, 0::2, :], [bk], [vk])
            kb.copy("dve", vst[vs][:, 1::2, 64:128], pv[:, 1::2, :], [bk], [vk])
            kt = q0 + sub
            kb.dma("sp", g.v1loc[kt], vst[vs][:].rearrange("p h d -> p (h d)"), [vk], [], "c2_vout%d" % vs)
            if kt >= 1:
                kb.dma("sp", g.v1own[kt - 1], vst[vs][:].rearrange("p h d -> p (h d)"), [vk], [], "c2_vout%d" % vs)
    kb.pop(ph)


def phase_D1(kb, g):
    ph = kb.push()
    uext = ph.sb("d1_uext", [128, 4, 528])
    A = ph.sb("d1_A", [128, 528])
    Bt = ph.sb("d1_B", [128, 528])
    rc = ph.sb("d1_rc", [128, 4, 512])
    pooled = [ph.sb("d1_pl%d" % i, [128, 512], BF16) for i in range(2)]
    pw = ph.sb("d1_pw", [128, 4, 128], BF16)
    psc = ph.sb("d1_psc", [128, 4])
    yst = [ph.sb("d1_yst%d" % i, [128, 512], BF16) for i in range(2)]
    kb.dma("pool", pw[:], g.pool_w.rearrange("g c d -> c g d"), [], ["d1_pw"], "d1_pw")
    kb.dma("sp", psc[:], g.pool_scale, [], ["d1_psc"], "d1_psc")
    nb = [0]
    for ti, (s, n) in enumerate(TT):
        kb.dma("sp", uext[:, :, 16:16 + n], g.uloc[:, :, s:s + n], [], ["d1_uext"], "d1_uext")
        if s >= 16:
            kb.dma("sp", uext[:, :, 0:16], g.uloc[:, :, s - 16:s], [], ["d1_uext"], "d1_uext")
            if s == HALO:
                kb.ts("dve", uext[:, :, 0:16], uext[:, :, 0:16], g.hv[:, 0:1], None, ALU.mult, None, ["d1_uext", "hv"], ["d1_uext"])
        else:
            kb.memset("dve", uext[:, :, 0:16], 0.0, ["d1_uext"])
        kb.dma("sp", rc[:, :, 0:n], g.rcnt[:, :, s:s + n], [], ["d1_rc"], "d1_rc")
        for gi in range(4):
            win = 2 << gi
            cur, ck = uext[:, gi, :], "d1_uext"
            bufs = [(A, "d1_A"), (Bt, "d1_B")]
            st = 1
            lo = 0
            k = 0
            while st < win:
                dst, dk = bufs[k % 2]
                k += 1
                nlo = lo + st
                kb.tt("dve", dst[:, nlo:16 + n], cur[:, nlo:16 + n], cur[:, nlo - st:16 + n - st], ALU.add, [ck], [dk])
                cur, ck = dst, dk
                lo = nlo
                st *= 2
            pl = gi % 2
            pk = "d1_pl%d" % pl
            other, ok_ = bufs[k % 2]
            kb.tt("dve", other[:, 16:16 + n], cur[:, 16:16 + n], rc[:, gi, 0:n], ALU.mult, [ck, "d1_rc"], [ok_])
            kb.tt("dve", pooled[pl][:, 0:n], other[:, 16:16 + n], uext[:, gi, 16:16 + n], ALU.subtract, [ok_, "d1_uext"], [pk])
            bi = nb[0] % 4
            nb[0] += 1
            ps, bk = g.bank[bi], "bk%d" % bi
            kb.mm(ps[:, 0:n], pw[:, gi, :], pooled[pl][:, 0:n], True, True, ["d1_pw", pk], [bk])
            yk = "d1_yst%d" % pl
            kb.act(yst[pl][:, 0:n], ps[:, 0:n], AF.Identity, [bk, "d1_psc"], [yk], scale=psc[:, gi:gi + 1], bias=0.0)
            kb.dma("sp", g.ycloc[:, gi, s:s + n], yst[pl][:, 0:n], [yk], [], "d1_yout%d" % pl)
    kb.pop(ph)


def phase_D2(kb, g, groups=None):
    ph = kb.push()
    qa = ph.sb("d2_qa", [97, 8, 256], BF16)
    ka = [ph.sb("d2_ka%d" % i, [97, 8, 128], BF16) for i in range(3)]
    va = [ph.sb("d2_va%d" % i, [128, 8, 128], BF16) for i in range(3)]
    pT = [ph.sb("d2_pT%d" % i, [128, 512], BF16) for i in range(3)]
    identb = ph.sb("d2_identb", [128, 128], BF16)
    psw = ph.sb("d2_psw", [128, 128])
    mD = ph.sb("d2_mD", [128, 128], BF16)
    mF = ph.sb("d2_mF", [128, 128], BF16)
    rec = ph.sb("d2_rec", [128, 512])
    rs = ph.sb("d2_rs", [128, 512])
    yst = ph.sb("d2_yst", [128, 256], BF16)
    kb.dma("sp", identb[:], g.identb, [], ["d2_identb"], "d2_c0")
    kb.dma("sp", psw[:], g.psw, [], ["d2_psw"], "d2_c1")
    kb.dma("sp", mD[:], g.maskD, [], ["d2_mD"], "d2_c2")
    kb.memset("dve", mF[:], -BIGS, ["d2_mF"])
    kb.memset("dve", rec[:], 0.0, ["d2_rec"])
    allg = [[0]] + [[2 * k - 1, 2 * k] for k in range(1, 9)]
    nka = [0]
    npt = [0]
    nS = [0]
    for gi, grp in enumerate(allg):
        if groups is not None and gi not in groups:
            continue
        NQ = 128 * len(grp)
        for qi_, t_ in enumerate(grp):
            kb.dma("sp", qa[0:96, :, qi_ * 128:(qi_ + 1) * 128], g.q1loc[t_], [], ["d2_qa"], "d2_qa%d" % qi_)
        kb.dma("sp", qa[96:97, :, :], g.onesrow.rearrange("p (h t) -> p h t", t=256), [], ["d2_qa"], "d2_qa2")
        a, b = grp[0], grp[-1]
        tiles = []
        for kt in range(128):
            tiles.append((g.k1g[kt * 96:(kt + 1) * 96, :], g.m1g[kt], g.v1g[kt * 128:(kt + 1) * 128, :], []))
        for j in range(b + 1):
            bias = []
            if len(grp) == 1:
                if j == a:
                    bias = [(0, mD, "d2_mD")]
            else:
                if j == a:
                    bias = [(0, mD, "d2_mD")]
                elif j == b:
                    bias = [(0, mF, "d2_mF"), (128, mD, "d2_mD")]
            tiles.append((g.k1loc[j], g.m1l[j], g.v1loc[j], bias))
        nt = len(tiles)
        pvq = []
        for ti, (ksrc, msrc, vsrc, bias) in enumerate(tiles):
            sl = nka[0] % 3
            nka[0] += 1
            kk, vk = "d2_ka%d" % sl, "d2_va%d" % sl
            kb.dma("sp", ka[sl][0:96], (r3(ksrc) if len(ksrc.shape) == 2 else ksrc), [], [kk], kk)
            kb.dma("sp", ka[sl][96:97], r3(msrc), [], [kk], kk + "m")
            kb.dma("sp", va[sl][:], r3(vsrc), [], [vk], vk)
            for hp in range(4):
                si = nS[0] % 3
                nS[0] += 1
                S_, sk = g.bank[si], "bk%d" % si
                for h2 in range(2):
                    h = hp * 2 + h2
                    c0 = h2 * NQ
                    kb.mm(S_[:, c0:c0 + NQ], ka[sl][0:97, h, :], qa[0:97, h, 0:NQ], h2 == 0, False, [kk, "d2_qa"], [sk])
                    for (bc, btab, bkey) in bias:
                        kb.mm(S_[:, c0 + bc:c0 + bc + 128], identb[:], btab[:], False, False, ["d2_identb", bkey], [sk])
                ps_ = npt[0] % 3
                npt[0] += 1
                pk = "d2_pT%d" % ps_
                kb.act(pT[ps_][:, 0:2 * NQ], S_[:, 0:2 * NQ], AF.Exp, [sk], [pk])
                def pv(hp=hp, sl=sl, ps_=ps_, vk=vk, pk=pk, ti=ti, NQ=NQ):
                    accb, ak = g.bank[4 + hp], "bk%d" % (4 + hp)
                    for h2 in range(2):
                        h = hp * 2 + h2
                        c0 = h2 * NQ
                        kb.mm(accb[:, c0:c0 + NQ], va[sl][:, h, :], pT[ps_][:, c0:c0 + NQ], (ti == 0 and h2 == 0), ti == nt - 1, [vk, pk], [ak])
                pvq.append(pv)
                if len(pvq) > 2:
                    pvq.pop(0)()
        while pvq:
            pvq.pop(0)()
        for hp in range(4):
            accb, ak = g.bank[4 + hp], "bk%d" % (4 + hp)
            kb.ts("dve", rec[64:128, 0:NQ], accb[64:128, 0:NQ], 1e-20, None, ALU.add, None, [ak], ["d2_rec"])
            kb.ts("dve", rec[0:64, NQ:2 * NQ], accb[0:64, NQ:2 * NQ], 1e-20, None, ALU.add, None, [ak], ["d2_rec"])
            kb.S.add("dve", lambda e, NQ=NQ: e.reciprocal(out=rec[64:128, 0:NQ], in_=rec[64:128, 0:NQ]), ["d2_rec"], ["d2_rec"])
            kb.S.add("dve", lambda e, NQ=NQ: e.reciprocal(out=rec[0:64, NQ:2 * NQ], in_=rec[0:64, NQ:2 * NQ]), ["d2_rec"], ["d2_rec"])
            shb, shk = g.bank[3], "bk3"
            kb.mm(shb[:, 0:2 * NQ], psw[:], rec[:, 0:2 * NQ], True, True, ["d2_psw", "d2_rec"], [shk])
            kb.copy("act", rs[:, 0:2 * NQ], shb[:, 0:2 * NQ], [shk], ["d2_rs"])
            kb.tt("dve", yst[0:64, 0:NQ], accb[0:64, 0:NQ], rs[0:64, 0:NQ], ALU.mult, [ak, "d2_rs"], ["d2_yst"])
            kb.tt("dve", yst[64:128, 0:NQ], accb[64:128, NQ:2 * NQ], rs[64:128, NQ:2 * NQ], ALU.mult, [ak, "d2_rs"], ["d2_yst"])
            kb.dma("sp", g.ydloc[:, hp, a * 128:a * 128 + NQ], yst[:, 0:NQ], ["d2_yst"], [], "d2_yout")
        if len(grp) == 1:
            kb.memset("dve", rec[:], 0.0, ["d2_rec"])
    kb.pop(ph)


EVEN_PERM = np.concatenate([np.arange(0, 2048), np.arange(2120, 3656), np.arange(2048, 2120)])


def build(phases, debug=(), ext_in=(), ext_out=()):
    kb = KB()
    kb.ext_in = set(ext_in)
    kb.ext_out = set(ext_out)
    g = G()
    g.kb = kb
    g.dbgB = "dbgB" in debug
    debug = [d_ for d_ in debug if d_ != "dbgB"]
    g.xT = kb.din("xT", [D, LT])
    g.hv = None
    g.c = kb.din("c", [128, 8])
    need = lambda *p: any(q in phases for q in p)
    g.ada_w = [kb.din("ada_w%d" % l, [D, 6 * D]) for l in range(2)] if need("mod") else None
    g.ada_b = [kb.din("ada_b%d" % l, [1, 6 * D]) for l in range(2)] if need("mod") else None
    g.ev_w_in = kb.din("ev_w_in", [D, 3656]) if need("A") else None
    g.ev_conv_w = kb.din("ev_conv_w", [128, 4, 3])
    hv_d = kb.din("hv", [128, 1])
    g.qloc = kb.dint("qloc", [NQT, 64, 8, 128], BF16)
    g.kloc = kb.dint("kloc", [NQT, 64, 8, 128], BF16)
    g.qiloc = kb.dint("qiloc", [NQT, 64, 8, 128], BF16)
    g.vloc = kb.dint("vloc", [NQT, 128, 1024], BF16)
    g.kiloc = kb.dint("kiloc", [64, LT], BF16)
    g.wloc = kb.dint("wloc", [NQT, 128, 8], F32)
    g.ybloc = kb.dint("ybloc", [128, 4, LT], BF16)
    g.yaloc = kb.dint("yaloc", [128, 4, LT], BF16)
    g.kown = kb.dint("kown", [16, 64, 8, 128], BF16)
    g.vown = kb.dint("vown", [16, 128, 1024], BF16)
    g.kiown = kb.dint("kiown", [64, OWN], BF16)
    g.kg = kb.dint("kg", [NCORES * 16 * 64, 1024], BF16)
    g.vg = kb.dint("vg", [NCORES * 16 * 128, 1024], BF16)
    g.kig = kb.dint("kig", [NCORES * 64, OWN], BF16)
    g.identb = kb.din("identb", [128, 128], BF16)
    g.psw = kb.din("psw", [128, 128])
    g.dtab = kb.din("dtab", [128, 8, 128], BF16)
    g.kcg = kb.din("kcg", [128, 4, 1024], BF16) if need("B") else None
    g.kcl = kb.din("kcl", [NQT, 4, 1024], BF16)
    g.qcst = kb.din("qcst", [NQT, 4, 1024], BF16)
    g.mrowg = kb.din("mrowg", [1, SEQ], BF16)
    g.mrowl = kb.din("mrowl", [1, LT], BF16)
    g.ev_w_out = kb.din("ev_w_out", [D, D])
    g.ffn_w_up = [(kb.din("ffn_w_up%d" % l, [D, 2 * DFF]) if need("F%d" % l) else None) for l in range(2)]
    g.ffn_w_down = [(kb.din("ffn_w_down%d" % l, [DFF, D]) if need("F%d" % l) else None) for l in range(2)]
    g.ffn_conv_w = [kb.din("ffn_conv_w%d" % l, [128, 44, 3]) for l in range(2)]
    lnp_d = kb.din("lnp", [128, 8, 8])
    g.od_w_in = kb.din("od_w_in", [D, 1216])
    g.w_uq2 = kb.din("w_uq2", [384, 1536])
    g.w_ukv2 = kb.din("w_ukv2", [256, 1024])
    g.latg = kb.din("latg", [128, 5])
    g.cosq = kb.din("cosq", [96, LT])
    g.sinq = kb.din("sinq", [96, LT])
    g.cosk = kb.din("cosk", [32, LT])
    g.sink = kb.din("sink", [32, LT])
    g.pool_w = kb.din("pool_w", [4, 128, 128])
    g.pool_scale = kb.din("pool_scale", [128, 4])
    g.rcnt = kb.din("rcnt", [128, 4, LT]) if need("D1") else None
    g.od_w_out = kb.din("od_w_out", [D, D])
    g.maskD = kb.din("maskD", [128, 128], BF16)
    g.onesrow = kb.din("onesrow", [1, 2048], BF16)
    g.m1g = kb.din("m1g", [128, 1, 1024], BF16)
    g.m1l = kb.din("m1l", [NQT, 1, 1024], BF16)
    g.uloc = kb.dint("uloc", [128, 4, LT], F32)
    g.q1loc = kb.dint("q1loc", [NQT, 96, 8, 128], BF16)
    g.k1loc = kb.dint("k1loc", [NQT, 96, 8, 128], BF16)
    g.k1own = kb.dint("k1own", [16, 96, 8, 128], BF16)
    g.v1loc = kb.dint("v1loc", [NQT, 128, 1024], BF16)
    g.v1own = kb.dint("v1own", [16, 128, 1024], BF16)
    g.k1g = kb.dint("k1g", [NCORES * 16 * 96, 1024], BF16)
    g.v1g = kb.dint("v1g", [NCORES * 16 * 128, 1024], BF16)
    g.ycloc = kb.dint("ycloc", [128, 4, LT], BF16)
    g.ydloc = kb.dint("ydloc", [128, 4, LT], BF16)
    g.outT = kb.dout("outT", [D, OWN])
    g.bank = [kb.psum("bank%d" % i) for i in range(8)]
    g.modT = [kb.sb("modT%d" % l, [128, 48]) for l in range(2)]
    g.mod1 = [kb.sb("mod1%d" % l, [128, 48]) for l in range(2)]
    g.hv = kb.sb("hv_sb", [128, 1])
    kb.dma("sp", g.hv[:], hv_d, [], ["hv"], "hv")
    lnp_sb = kb.sb("lnp_sb", [128, 8, 8])
    kb.dma("sp", lnp_sb[:], lnp_d, [], ["lnp"], "lnp")
    g.lnp = {n: lnp_sb[:, i, :] for i, n in enumerate(["mix0g", "mix0b", "ffn0g", "ffn0b", "mix1g", "mix1b", "ffn1g", "ffn1b"])}

    if "mod" in phases:
        phase_mod(kb, g)
    if "modout" in phases:
        for l in range(2):
            o = kb.dout("modT%d_x" % l, [128, 48])
            kb.final_ops.append(kb.dma("sp", o, g.modT[l][:], ["modT%d" % l], [], "modout%d" % l))
    if "modin" in phases:
        for l in range(2):
            i_ = kb.din("modT%d_x" % l, [128, 48])
            kb.dma("sp", g.modT[l][:], i_, [], ["modT%d" % l], "modin%d" % l)
            kb.ts("dve", g.mod1[l][:], g.modT[l][:], 1.0, None, ALU.add, None, ["modT%d" % l], ["mod1%d" % l])
    if "A" in phases:
        phase_A(kb, g)
    if "G1" in phases:
        allgather(kb, g.kown.rearrange("k d h t -> (k d) (h t)"), g.kg, "x", "x", "cc")
        allgather(kb, g.vown.rearrange("k s f -> (k s) f"), g.vg, "x", "x", "cc")
        allgather(kb, g.kiown, g.kig, "x", "x", "cc")
        kb.S.barrier()
    if "B" in phases:
        phase_B(kb, g, qtiles=QTILES)
    if "yain" in phases:
        yain = kb.din("yain", [128, 4, LT], BF16)
        kb.dma("sp", g.yaloc, yain, [], [], "yain")
        kb.S.barrier()
    g.xres = kb.sb("xres", [128, 8, LT])
    if "xin" in phases:
        xi = kb.din("xres_x", [128, 8, LT])
        kb.dma("sp", g.xres[:], xi, [], ["xres"], "xin")
    if "B2" in phases:
        phase_B2(kb, g)
    if "F0" in phases:
        phase_FFN(kb, g, 0)
    if "C2" in phases:
        phase_C2(kb, g)
    if "xout" in phases:
        xo = kb.dout("xres_x", [128, 8, LT])
        kb.final_ops.append(kb.dma("sp", xo, g.xres[:], ["xres"], [], "xout"))
    if "G2" in phases:
        allgather(kb, g.k1own.rearrange("k d h t -> (k d) (h t)"), g.k1g, "x", "x", "cc")
        allgather(kb, g.v1own.rearrange("k s f -> (k s) f"), g.v1g, "x", "x", "cc")
        kb.S.barrier()
    if "D1" in phases:
        phase_D1(kb, g)
    if "D2" in phases:
        phase_D2(kb, g, groups=MGROUPS)
    if "D3" in phases:
        phase_B2(kb, g, 1)
    if "F1" in phases:
        phase_FFN(kb, g, 1)
    if "out" in phases:
        kb.final_ops.append(kb.dma("sp", fm(g.outT), g.xres[:, :, HALO:LT], ["xres"], [], "outT"))

    last = None
    for name in debug:
        if name == "xres":
            o = kb.dout("dbg_xres", [128, 8, LT])
            last = kb.dma("sp", o, g.xres[:], ["xres"], [], "dbg")
        elif name in ("modT0", "modT1"):
            o = kb.dout("dbg_" + name, [128, 48])
            last = kb.dma("sp", o, g.modT[int(name[-1])][:], [name], [], "dbg")
        else:
            src = getattr(g, name)
            shp = list(src.shape)
            o = kb.dout("dbg_" + name, shp, src.dtype)
            last = kb.dma("sp", o, src, [], [], "dbg")
        kb.final_ops.append(last)
    if kb.scr_out:
        kb.final_ops.extend(list(kb.S.last_dma_on_key.values()))
    nc = kb.finish()
    return nc, kb


def prep_inputs(inputs):
    x = np.asarray(inputs["x"], np.float32)[0]
    com = {}
    com["c"] = np.ascontiguousarray(np.asarray(inputs["c"], np.float32).reshape(128, 8))
    for l in range(2):
        com["ada_w%d" % l] = np.ascontiguousarray(np.asarray(inputs["ada_w"], np.float32)[l])
        com["ada_b%d" % l] = np.ascontiguousarray(np.asarray(inputs["ada_b"], np.float32)[l].reshape(1, 6 * D))
    com["ev_w_in"] = np.ascontiguousarray(np.asarray(inputs["ev_w_in"], np.float32)[0][:, EVEN_PERM])
    com["ev_conv_w"] = np.ascontiguousarray(
        np.asarray(inputs["ev_conv_w"], np.float32)[0].reshape(3, 4, 128).transpose(2, 1, 0))
    com["ev_w_out"] = np.ascontiguousarray(np.asarray(inputs["ev_w_out"], np.float32)[0])
    for l in range(2):
        com["ffn_w_up%d" % l] = np.ascontiguousarray(np.asarray(inputs["ffn_w_up"], np.float32)[l])
        com["ffn_w_down%d" % l] = np.ascontiguousarray(np.asarray(inputs["ffn_w_down"], np.float32)[l])
        com["ffn_conv_w%d" % l] = np.ascontiguousarray(
            np.asarray(inputs["ffn_conv_w"], np.float32)[l].reshape(3, 44, 128).transpose(2, 1, 0))
    lnp = np.zeros((128, 8, 8), np.float32)
    for i_, (nm, l) in enumerate([("ln_mix_g", 0), ("ln_mix_b", 0), ("ln_ffn_g", 0), ("ln_ffn_b", 0),
                                  ("ln_mix_g", 1), ("ln_mix_b", 1), ("ln_ffn_g", 1), ("ln_ffn_b", 1)]):
        lnp[:, i_, :] = np.asarray(inputs[nm], np.float32)[l].reshape(8, 128).T
    com["lnp"] = lnp
    od = np.asarray(inputs["od_w_in"], np.float32)[0]
    com["od_w_in"] = np.ascontiguousarray(np.concatenate([od[:, 0:1184], od[:, 1168:1184], od[:, 1152:1168]], 1))
    wuq = np.asarray(inputs["w_uq"], np.float32)[0].reshape(384, 8, 96)
    wuq_sw = np.concatenate([wuq[:, :, 0:64], wuq[:, :, 80:96], wuq[:, :, 64:80]], 2)
    com["w_uq2"] = np.ascontiguousarray(np.concatenate([wuq.reshape(384, 768), wuq_sw.reshape(384, 768)], 1))
    wukv = np.asarray(inputs["w_ukv"], np.float32)[0].reshape(256, 8, 128)
    com["w_ukv2"] = np.ascontiguousarray(np.concatenate([wukv[:, :, 0:64].reshape(256, 512), wukv[:, :, 64:128].reshape(256, 512)], 1))
    latg = np.zeros((128, 5), np.float32)
    latg[:, 0:3] = np.asarray(inputs["q_norm_g"], np.float32)[0].reshape(3, 128).T
    latg[:, 3:5] = np.asarray(inputs["kv_norm_g"], np.float32)[0].reshape(2, 128).T
    com["latg"] = latg
    com["pool_w"] = np.ascontiguousarray(np.asarray(inputs["pool_w"], np.float32)[0])
    com["pool_scale"] = np.ascontiguousarray(np.asarray(inputs["pool_scale"], np.float32)[0].reshape(4, 128).T)
    com["od_w_out"] = np.ascontiguousarray(np.asarray(inputs["od_w_out"], np.float32)[0])
    sp2, tp2 = np.meshgrid(np.arange(128), np.arange(128), indexing="ij")
    com["maskD"] = (np.where((sp2 >= 64) & (tp2 < 64), -BIGS, 0.0)).astype(np.float32).astype(NPBF)
    com["onesrow"] = np.ones((1, 2048), np.float32).astype(NPBF)
    inv = (np.float32(10000.0) ** (-np.arange(0, 32, 2, dtype=np.float32) / np.float32(32))).astype(np.float32)
    qscale = np.float32(96.0 ** -0.5)
    slopes = np.array([2.0 ** (-(h + 1)) for h in range(8)], np.float64)
    com["identb"] = np.eye(128, dtype=np.float32).astype(NPBF)
    psw = np.zeros((128, 128), np.float32)
    for m_ in range(128):
        psw[(m_ + 64) % 128, m_] = 1.0
    com["psw"] = psw
    sp_, tp_ = np.meshgrid(np.arange(128), np.arange(128), indexing="ij")
    base = np.maximum(sp_ - tp_, 0) * ((sp_ // 64) == (tp_ // 64))
    com["dtab"] = np.stack([-2.0 * slopes[h] * base for h in range(8)], 1).astype(np.float32).astype(NPBF)

    def krows(kt):
        r = np.zeros((4, 8, 128), np.float64)
        for h in range(8):
            r[0, h, :] = -128.0 * slopes[h]
            r[1, h, :] = -slopes[h]
            r[2, h, :] = 128.0 * slopes[h] * kt
            r[3, h, :] = slopes[h] * np.arange(128)
        return r.reshape(4, 1024)

    def qrows(a):
        r = np.zeros((4, 8, 128), np.float64)
        r[0] = a
        r[1] = np.arange(128)[None, :]
        r[2] = 1.0
        r[3] = 1.0
        return r.reshape(4, 1024)

    com["kcg"] = np.stack([krows(kt) for kt in range(128)], 0).astype(np.float32).astype(NPBF)
    maps = []
    for cidx in range(NCORES):
        m = dict(com)
        t0 = cidx * OWN
        a0 = (t0 - HALO) // 128
        m["kcl"] = np.stack([krows(a0 + j) for j in range(NQT)], 0).astype(np.float32).astype(NPBF)
        m["qcst"] = np.stack([qrows(a0 + j) for j in range(NQT)], 0).astype(np.float32).astype(NPBF)
        mg = np.zeros((1, SEQ), np.float32)
        mg[0, max(t0 - HALO, 0):] = -BIGI
        m["mrowg"] = mg.astype(NPBF)
        ml = np.zeros((1, LT), np.float32)
        if cidx == 0:
            ml[0, 0:HALO] = -BIGI
        m["mrowl"] = ml.astype(NPBF)
        pos = np.maximum(np.arange(LT) + (t0 - HALO), 0)
        ang = (pos.astype(np.float32)[:, None] * inv[None, :]).astype(np.float32)
        cs, sn = np.cos(ang).T.astype(np.float32), np.sin(ang).T.astype(np.float32)
        cosq = np.ones((96, LT), np.float32)
        sinq = np.zeros((96, LT), np.float32)
        cosq[64:80] = cs
        cosq[80:96] = cs
        sinq[64:80] = -sn
        sinq[80:96] = sn
        m["cosq"] = cosq * qscale
        m["sinq"] = sinq * qscale
        m["cosk"] = np.concatenate([cs, cs], 0)
        m["sink"] = np.concatenate([-sn, sn], 0)
        rc = np.zeros((128, 4, LT), np.float32)
        for gi_, win in enumerate((2, 4, 8, 16)):
            rc[:, gi_, :] = (1.0 / np.minimum(pos + 1, win).astype(np.float32))[None, :]
        m["rcnt"] = rc
        m1g = np.zeros((128, 1, 1024), np.float32)
        for kt in range(128):
            if kt * 128 >= t0 - HALO:
                m1g[kt] = -BIGS
        m["m1g"] = m1g.astype(NPBF)
        m1l = np.zeros((NQT, 1, 1024), np.float32)
        if cidx == 0:
            m1l[0] = -BIGS
        m["m1l"] = m1l.astype(NPBF)
        xt = np.zeros((D, LT), np.float32)
        lo = t0 - HALO
        if lo < 0:
            xt[:, HALO:] = x[t0:t0 + OWN].T
        else:
            xt[:] = x[lo:t0 + OWN].T
        m["xT"] = xt
        m["hv"] = np.full((128, 1), 0.0 if cidx == 0 else 1.0, np.float32)
        maps.append(m)
    return maps


ALL_PHASES = ["mod", "A", "G1", "B", "B2", "F0", "C2", "G2", "D1", "D2", "D3", "F1", "out"]
FUSED = True
L1_OUT = ["kown", "vown", "kiown", "qloc", "kloc", "qiloc", "vloc", "kiloc", "wloc", "ybloc"]
L2_OUT = ["uloc", "q1loc", "k1loc", "v1loc", "k1own", "v1own"]


def _run(nc, kb, maps):
    maps = [{k: m[k] for k in kb.in_names} for m in maps]
    return run_bass_kernel_spmd(nc, maps, core_ids=list(range(NCORES))).results


def kernel(**inputs):
    maps = prep_inputs(inputs)
    if FUSED:
        nc, kb = build(ALL_PHASES)
        res = _run(nc, kb, maps)
    else:
        nc, kb = build(["mod", "A", "modout"], ext_out=L1_OUT)
        r1 = _run(nc, kb, maps)
        cat = lambda n, r: np.concatenate([np.asarray(r[c][n]).reshape(-1, np.asarray(r[c][n]).shape[-1] if n == "kiown" else 1024)
                                           for c in range(NCORES)], 0)
        kg, vg, kig = cat("kown", r1), cat("vown", r1), cat("kiown", r1)
        for c in range(NCORES):
            for n in ["qloc", "kloc", "qiloc", "vloc", "kiloc", "wloc", "ybloc", "modT0_x", "modT1_x"]:
                maps[c][n] = np.asarray(r1[c][n])
            maps[c]["kg"], maps[c]["vg"], maps[c]["kig"] = kg, vg, kig
        nc, kb = build(["modin", "B", "B2", "F0", "C2", "xout"], ext_in=["qloc", "kloc", "qiloc", "vloc", "kiloc", "wloc", "ybloc", "kg", "vg", "kig"],
                       ext_out=L2_OUT)
        r2 = _run(nc, kb, maps)
        k1g, v1g = cat("k1own", r2), cat("v1own", r2)
        for c in range(NCORES):
            for n in ["uloc", "q1loc", "k1loc", "v1loc", "xres_x"]:
                maps[c][n] = np.asarray(r2[c][n])
            maps[c]["k1g"], maps[c]["v1g"] = k1g, v1g
        nc, kb = build(["modin", "xin", "D1", "D2", "D3", "F1", "out"], ext_in=["uloc", "q1loc", "k1loc", "v1loc", "k1g", "v1g"])
        res = _run(nc, kb, maps)
    out = np.empty((1, SEQ, D), np.float32)
    for c in range(NCORES):
        out[0, c * OWN:(c + 1) * OWN, :] = np.asarray(res[c]["outT"], np.float32).T
    return out
```
